# Optimizing a Trainium2 kernel written in Bass

```python
import math
import jax, jax.numpy as jnp
from jax import lax
import numpy as np

D_MODEL = 1024
BATCH = 8
SEQ = 2048
DEPTH = 2

N_A_LAYERS = DEPTH // 2
N_B_LAYERS = DEPTH - N_A_LAYERS
HEAD_DIM = 64
RWKV_HEADS = D_MODEL // HEAD_DIM
DECAY_LORA = 64
AAA_LORA = 64
GATE_LORA = 160
ATTN_HEADS = D_MODEL // HEAD_DIM
KV_HEADS = 4
GROUP = ATTN_HEADS // KV_HEADS
WINDOW = 128
BLOCK = 128
D_FF = 2816
CONV_WIDTH = 3
RMS_EPS = 1e-6
GN_EPS = 64e-5
NEG_INF = -1e30

kernel_name = 'yoco_rwkv7_swa_sink_convffn_trunk'


def rms_norm(x, g):
    xf = x.astype(jnp.float32)
    y = xf * lax.rsqrt(jnp.mean(xf * xf, axis=-1, keepdims=True) + RMS_EPS)
    return (y * g.astype(jnp.float32)).astype(x.dtype)


def conv_ffn(x, w_up, conv_w, conv_b, w_down):
    h = x @ w_up
    c = h.shape[-1]
    h = lax.conv_general_dilated(
        h, conv_w[:, None, :].astype(h.dtype), window_strides=(1,),
        padding=[(CONV_WIDTH - 1, 0)], dimension_numbers=('NWC', 'WIO', 'NWC'),
        feature_group_count=c) + conv_b
    a, u = jnp.split(h, 2, axis=-1)
    return (jax.nn.gelu(a, approximate=True) * u) @ w_down


def rwkv7_time_mix(x, mu, w_rkv, w_decay0, w_decay1, w_decay2, a0, a1, a2, g1, g2,
                   k_k, k_a, r_k, gn_g, gn_b, w_o):
    B, T, D = x.shape
    H, N = RWKV_HEADS, HEAD_DIM
    f32 = jnp.float32
    xx = jnp.pad(x, ((0, 0), (1, 0), (0, 0)))[:, :-1] - x
    xr, xw, xk, xv, xa, xg = [x + xx * mu[i] for i in range(6)]
    r = xr @ w_rkv[0]
    k = xk @ w_rkv[1]
    v = xv @ w_rkv[2]
    w = -jax.nn.softplus(-(w_decay0 + jnp.tanh(xw @ w_decay1) @ w_decay2)) - 0.5
    a = jax.nn.sigmoid(a0 + (xa @ a1) @ a2)
    g = jax.nn.sigmoid(xg @ g1) @ g2
    heads = lambda z: z.reshape(B, T, H, N).astype(f32)
    kk = heads(k * k_k)
    kk = kk * lax.rsqrt(jnp.maximum(jnp.sum(kk * kk, axis=-1, keepdims=True), 1e-24))
    k = k * (1 + (a - 1) * k_a)
    r_h, k_h, v_h, a_h = heads(r), heads(k), heads(v), heads(a)
    decay = jnp.exp(-jnp.exp(heads(w)))
    vec_a, vec_b = -kk, kk * a_h

    def step(S, inp):
        r_t, d_t, k_t, v_t, a_t, b_t = inp
        sa = jnp.einsum('bhij,bhj->bhi', S, a_t)
        S = (S * d_t[:, :, None, :] + sa[..., None] * b_t[:, :, None, :]
             + v_t[..., None] * k_t[:, :, None, :])
        return S, jnp.einsum('bhij,bhj->bhi', S, r_t)

    seq_first = lambda z: jnp.moveaxis(z, 1, 0)
    state0 = jnp.zeros((B, H, N, N), f32)
    _, y = lax.scan(step, state0, tuple(seq_first(z) for z in (r_h, decay, k_h, v_h, vec_a, vec_b)))
    y = jnp.moveaxis(y, 0, 1)
    mean = jnp.mean(y, axis=-1, keepdims=True)
    var = jnp.mean(jnp.square(y - mean), axis=-1, keepdims=True)
    y = ((y - mean) * lax.rsqrt(var + GN_EPS)).reshape(B, T, D)
    y = y * gn_g.astype(f32) + gn_b.astype(f32)
    bonus = jnp.sum(r_h * k_h * r_k.astype(f32), axis=-1, keepdims=True) * v_h
    y = y + bonus.reshape(B, T, D)
    return (y * g.astype(f32)).astype(x.dtype) @ w_o


def shared_kv(h, kv_g, w_kv):
    B, T, _ = h.shape
    kv = rms_norm(h, kv_g) @ w_kv
    k, v = jnp.split(kv, 2, axis=-1)
    return k.reshape(B, T, KV_HEADS, HEAD_DIM), v.reshape(B, T, KV_HEADS, HEAD_DIM)


def swa_sink_attention(x, k, v, w_q, sinks, w_o):
    B, T, D = x.shape
    nb = T // BLOCK
    q = (x @ w_q).reshape(B, nb, BLOCK, KV_HEADS, GROUP, HEAD_DIM)

    def band(z):
        zp = jnp.pad(z, ((0, 0), (BLOCK, 0), (0, 0), (0, 0))).reshape(B, nb + 1, BLOCK, KV_HEADS, HEAD_DIM)
        return jnp.concatenate([zp[:, :-1], zp[:, 1:]], axis=2)

    kb, vb = band(k), band(v)
    s = jnp.einsum('bnqhgd,bnkhd->bnhgqk', q, kb).astype(jnp.float32) * (HEAD_DIM ** -0.5)
    q_pos = jnp.arange(BLOCK)[:, None]
    k_pos = jnp.arange(2 * BLOCK)[None, :] - BLOCK
    rel = q_pos - k_pos
    in_window = (rel >= 0) & (rel < WINDOW)
    blk = jnp.arange(nb)[:, None, None]
    mask = in_window[None] & ((blk > 0) | (k_pos[None] >= 0))
    s = jnp.where(mask[None, :, None, None], s, NEG_INF)
    sk = sinks.astype(jnp.float32).reshape(1, 1, KV_HEADS, GROUP, 1, 1)
    m = jnp.maximum(jnp.max(s, axis=-1, keepdims=True), sk)
    p = jnp.exp(s - m)
    denom = jnp.sum(p, axis=-1, keepdims=True) + jnp.exp(sk - m)
    o = jnp.einsum('bnhgqk,bnkhd->bnqhgd', (p / denom).astype(vb.dtype), vb)
    return o.reshape(B, T, ATTN_HEADS * HEAD_DIM) @ w_o


def setup_inputs(seed: int = 0) -> dict:
    key = jax.random.key(seed)
    ks = jax.random.split(key, 27)
    nrm = lambda k, shape, scale: jax.random.normal(k, shape, jnp.float32) * scale
    D, NA, NB = D_MODEL, N_A_LAYERS, N_B_LAYERS
    return {
        'x': nrm(ks[0], (BATCH, SEQ, D), 1.0),
        'norm_g': 1.0 + nrm(ks[1], (DEPTH, 4, D), 0.05),
        'mu': jax.random.uniform(ks[2], (NA, 6, D), jnp.float32),
        'w_rkv': nrm(ks[3], (NA, 3, D, D), D ** -0.5),
        'w_decay0': jax.random.uniform(ks[4], (NA, D), jnp.float32, -6.5, -1.5),
        'w_decay1': nrm(ks[5], (NA, D, DECAY_LORA), D ** -0.5),
        'w_decay2': nrm(ks[6], (NA, DECAY_LORA, D), 0.1 * DECAY_LORA ** -0.5),
        'a0': nrm(ks[7], (NA, D), 0.1),
        'a1': nrm(ks[8], (NA, D, AAA_LORA), D ** -0.5),
        'a2': nrm(ks[9], (NA, AAA_LORA, D), 0.5 * AAA_LORA ** -0.5),
        'g1': nrm(ks[10], (NA, D, GATE_LORA), D ** -0.5),
        'g2': nrm(ks[11], (NA, GATE_LORA, D), GATE_LORA ** -0.5),
        'k_k': 0.85 + nrm(ks[12], (NA, D), 0.05),
        'k_a': 1.0 + nrm(ks[13], (NA, D), 0.05),
        'r_k': nrm(ks[14], (NA, RWKV_HEADS, HEAD_DIM), 0.1),
        'gn_g': 1.0 + nrm(ks[15], (NA, D), 0.05),
        'gn_b': nrm(ks[16], (NA, D), 0.01),
        'w_o_rwkv': nrm(ks[17], (NA, D, D), D ** -0.5),
        'kv_g': 1.0 + nrm(ks[18], (D,), 0.05),
        'w_kv': nrm(ks[19], (D, 2 * KV_HEADS * HEAD_DIM), D ** -0.5),
        'w_q': nrm(ks[20], (NB, D, ATTN_HEADS * HEAD_DIM), D ** -0.5),
        'sinks': nrm(ks[21], (NB, ATTN_HEADS), 0.5),
        'w_o_attn': nrm(ks[22], (NB, ATTN_HEADS * HEAD_DIM, D), (ATTN_HEADS * HEAD_DIM) ** -0.5),
        'w_up': nrm(ks[23], (DEPTH, D, 2 * D_FF), D ** -0.5),
        'conv_w': nrm(ks[24], (DEPTH, CONV_WIDTH, 2 * D_FF), CONV_WIDTH ** -0.5),
        'conv_b': nrm(ks[25], (DEPTH, 2 * D_FF), 0.01),
        'w_down': nrm(ks[26], (DEPTH, D_FF, D), D_FF ** -0.5),
    }


def reference(x, norm_g, mu, w_rkv, w_decay0, w_decay1, w_decay2, a0, a1, a2, g1, g2,
              k_k, k_a, r_k, gn_g, gn_b, w_o_rwkv, kv_g, w_kv, w_q, sinks, w_o_attn,
              w_up, conv_w, conv_b, w_down):
    k_sh = v_sh = None
    for l in range(DEPTH):
        h = rms_norm(x, norm_g[l, 0])
        if l < N_A_LAYERS:
            i = l
            h = rwkv7_time_mix(h, mu[i], w_rkv[i], w_decay0[i], w_decay1[i], w_decay2[i],
                               a0[i], a1[i], a2[i], g1[i], g2[i], k_k[i], k_a[i], r_k[i],
                               gn_g[i], gn_b[i], w_o_rwkv[i])
        else:
            j = l - N_A_LAYERS
            h = swa_sink_attention(h, k_sh, v_sh, w_q[j], sinks[j], w_o_attn[j])
        x = x + rms_norm(h, norm_g[l, 1])
        h = conv_ffn(rms_norm(x, norm_g[l, 2]), w_up[l], conv_w[l], conv_b[l], w_down[l])
        x = x + rms_norm(h, norm_g[l, 3])
        if l == N_A_LAYERS - 1:
            k_sh, v_sh = shared_kv(x, kv_g, w_kv)
    return x
```

```python
import numpy as np
import concourse.bass as bass
import concourse.mybir as mybir
from contextlib import ExitStack
from concourse.bass_utils import run_bass_kernel_spmd

F32 = mybir.dt.float32
BF16 = mybir.dt.bfloat16
AF = mybir.ActivationFunctionType
ALU = mybir.AluOpType
AX = mybir.AxisListType

T, D, DFF = 2048, 1024, 2816
NSLOT = 24
NCST = 1352
NPAR = 544
C0 = float(np.exp(-0.5))
import os
CUT = int(os.environ.get('KCUT', '99'))


class Op:
    __slots__ = ("eng", "fn", "deps", "sig", "sigval", "dma", "slot", "idx")


class Sched:
    def __init__(self):
        self.ops = []
        self.last_w = {}
        self.readers = {}
        self.ndma = 0
        self.ndma_sw = 0
        self.slot_last = [None] * NSLOT
        self.slot_uses = [0] * NSLOT
        self.last_eng = {}
        self.pending = {}

    def barrier(self):
        s = set(self.last_eng.values())
        for x in self.slot_last:
            if x is not None:
                s.add(x)
        for e in ["pe", "act", "dve", "pool", "sp"]:
            self.pending[e] = set(s) | self.pending.get(e, set())

    def add(self, eng, fn, reads=(), writes=(), dma=False):
        op = Op()
        op.eng, op.fn, op.dma, op.sig, op.sigval, op.slot = eng, fn, dma, False, None, None
        op.idx = len(self.ops)
        deps = set()
        for k in reads:
            w = self.last_w.get(k)
            if w is not None:
                deps.add(w)
            if isinstance(k, tuple) and k[0] == "ps":
                for r in self.readers.get(k, ()):
                    if self.ops[r].eng != eng:
                        deps.add(r)
        for k in writes:
            w = self.last_w.get(k)
            if w is not None:
                deps.add(w)
            for r in self.readers.get(k, ()):
                deps.add(r)
        for k in reads:
            self.readers.setdefault(k, []).append(op.idx)
        for k in writes:
            self.last_w[k] = op.idx
            self.readers[k] = []
        if eng in self.pending:
            deps |= self.pending.pop(eng)
        if dma:
            if eng == "pool":
                s = NSLOT // 2 + self.ndma_sw % (NSLOT // 2)
                self.ndma_sw += 1
            else:
                s = self.ndma % (NSLOT // 2)
                self.ndma += 1
            if self.slot_last[s] is not None:
                deps.add(self.slot_last[s])
            self.slot_last[s] = op.idx
            self.slot_uses[s] += 1
            op.slot = s
            op.sigval = 16 * self.slot_uses[s]
            op.sig = True
        else:
            self.last_eng[eng] = op.idx
        deps.discard(op.idx)
        op.deps = sorted(deps)
        self.ops.append(op)
        return op

    def emit(self, nc, es):
        ops = self.ops
        for o in ops:
            for d in o.deps:
                p = ops[d]
                if p.dma:
                    continue
                if p.eng == "pe" and o.eng == "pe" and not o.dma:
                    continue
                p.sig = True
        cnt = {}
        for o in ops:
            if o.dma or not o.sig:
                continue
            cnt[o.eng] = cnt.get(o.eng, 0) + 1
            o.sigval = cnt[o.eng]
        engs = ["pe", "act", "dve", "pool", "sp"]
        esem = {e: es.enter_context(nc.semaphore("s_" + e)) for e in engs if e != "sp"}
        dsem = [es.enter_context(nc.semaphore("d_%d" % i)) for i in range(NSLOT)]
        block = es.enter_context(nc.Block())
        streams = {e: [o for o in ops if o.eng == e] for e in engs}

        def run(e, engine):
            waited = {}
            for o in streams[e]:
                for d in o.deps:
                    p = ops[d]
                    if p.dma:
                        key, sem, val = ("d", p.slot), dsem[p.slot], p.sigval
                    else:
                        if p.eng == "pe" and e == "pe" and not o.dma:
                            continue
                        key, sem, val = ("e", p.eng), esem[p.eng], p.sigval
                    if waited.get(key, 0) >= val:
                        continue
                    waited[key] = val
                    engine.wait_ge(sem, val)
                ins = o.fn(engine)
                if o.dma:
                    ins.then_inc(dsem[o.slot], 16)
                elif o.sig:
                    ins.then_inc(esem[e], 1)
            if e == "sp":
                for s in range(NSLOT):
                    if self.slot_uses[s] and waited.get(("d", s), 0) < 16 * self.slot_uses[s]:
                        engine.wait_ge(dsem[s], 16 * self.slot_uses[s])

        @block.tensor
        def _(eng):
            run("pe", eng)

        @block.scalar
        def _(eng):
            run("act", eng)

        @block.vector
        def _(eng):
            run("dve", eng)

        @block.gpsimd
        def _(eng):
            run("pool", eng)

        @block.sync
        def _(eng):
            run("sp", eng)


OFF_X = 0
OFF_H = 65536
OFF_Z = 98432
OFF_W = 131200
ASZ = 202240


class KB:
    def __init__(self, nc, es, dram):
        self.nc, self.es, self.S, self.d = nc, es, Sched(), dram
        self.arena = es.enter_context(nc.sbuf_tensor("arena", [128, ASZ // 4], F32))
        self.cst = es.enter_context(nc.sbuf_tensor("cst_sb", [128, NCST], F32))
        self.cstb = es.enter_context(nc.sbuf_tensor("cstb_sb", [128, NCST], BF16))
        self.par = es.enter_context(nc.sbuf_tensor("par_sb", [128, NPAR + 56], F32))
        self.pp = [es.enter_context(nc.psum_tensor("pp%d" % i, [128, 1024], F32)) for i in range(4)]
        self.wtop = OFF_W
        self.xT = self.f32(OFF_X, 8 * T).rearrange("p (c t) -> p c t", c=8)
        self.hT = self.bf(OFF_H, 8 * 2049).rearrange("p (c t) -> p c t", c=8)
        self.zT = self.bf(OFF_Z, 8 * T).rearrange("p (c t) -> p c t", c=8)
        self.uid = 0

    def f32(self, off, n):
        assert off % 4 == 0 and off + 4 * n <= ASZ, (off, n)
        return self.arena[:, off // 4: off // 4 + n]

    def bf(self, off, n):
        assert off % 4 == 0 and n % 2 == 0 and off + 2 * n <= ASZ, (off, n)
        return self.arena[:, off // 4: off // 4 + n // 2].bitcast(BF16)

    def wreset(self, off=OFF_W):
        self.wtop = off

    def walloc(self, n, dt, lim=ASZ):
        sz = n * (4 if dt == F32 else 2)
        sz = (sz + 63) // 64 * 64
        off = self.wtop
        self.wtop += sz
        assert self.wtop <= lim, ("arena overflow", self.wtop, lim)
        return self.f32(off, n) if dt == F32 else self.bf(off, n)

    def bank(self, b, lo=0, hi=512):
        return self.pp[b // 2][:, (b % 2) * 512 + lo: (b % 2) * 512 + hi]

    def bankb(self, b, lo=0, hi=1024):
        v = self.pp[b // 2][:, (b % 2) * 512: (b % 2) * 512 + 512].bitcast(BF16)
        return v[:, lo:hi]

    def pcol(self, v, c=None, n=8):
        if c is None:
            return self.par[:, v * 8: v * 8 + n]
        return self.par[:, v * 8 + c: v * 8 + c + 1]

    def mm(self, out, lhsT, rhs, start, stop, R, W):
        self.S.add("pe", lambda e: e.matmul(out, lhsT, rhs, start=start, stop=stop), reads=R, writes=W)

    def tr(self, out, in_, ident, R, W):
        self.S.add("pe", lambda e: e.transpose(out, in_, ident), reads=R, writes=W)

    def act(self, out, in_, func, R, W, bias=None, scale=None, accum_out=None, eng="act"):
        kw = {}
        if bias is not None:
            kw["bias"] = bias
        if scale is not None:
            kw["scale"] = scale
        if accum_out is not None:
            kw["accum_out"] = accum_out
        self.S.add("act", lambda e: e.activation(out, in_, func, **kw), reads=R, writes=W)

    def tt(self, eng, out, in0, in1, op, R, W):
        self.S.add(eng, lambda e: e.tensor_tensor(out, in0, in1, op), reads=R, writes=W)

    def ts(self, eng, out, in0, s1, s2, op0, op1, R, W):
        if op1 is None:
            self.S.add(eng, lambda e: e.tensor_scalar(out, in0, s1, None, op0), reads=R, writes=W)
        else:
            self.S.add(eng, lambda e: e.tensor_scalar(out, in0, s1, s2, op0, op1), reads=R, writes=W)

    def stt(self, out, in0, scalar, in1, op0, op1, R, W):
        self.S.add("dve", lambda e: e.scalar_tensor_tensor(out, in0, scalar, in1, op0, op1), reads=R, writes=W)

    def cp(self, eng, out, in_, R, W):
        if eng == "act":
            self.S.add("act", lambda e: e.activation(out, in_, AF.Copy), reads=R, writes=W)
        else:
            self.S.add(eng, lambda e: e.tensor_copy(out, in_), reads=R, writes=W)

    def recip(self, out, in_, R, W):
        self.S.add("dve", lambda e: e.reciprocal(out, in_), reads=R, writes=W)

    def memset(self, eng, ap, val, W):
        self.S.add(eng, lambda e: e.memset(ap, val), writes=W)

    def dma(self, q, out, in_, R, W):
        self.S.add(q, lambda e: e.dma_start(out=out, in_=in_), reads=R, writes=W, dma=True)

    def setup(self):
        d = self.d
        self.dma("sp", self.cst[:], d["cst"][:, :], [], ["cst"])
        self.dma("sp", self.par[:, 0:NPAR], d["par"][:, :], [], ["par"])
        self.cp("dve", self.cstb[:], self.cst[:], ["cst"], ["cstb"])
        self.ts("dve", self.par[:, NPAR:NPAR + 48], self.par[:, 64:112], -1.0, 1.0, ALU.mult, ALU.add, ["par"], ["par2"])
        self.ts("dve", self.par[:, NPAR + 48:NPAR + 56], self.par[:, 136:144], -1.0, 1.0, ALU.mult, ALU.add, ["par"], ["par2"])
        self.ident = self.cst[:, 0:128]
        self.identb = self.cstb[:, 0:128]
        self.onesb = self.cstb[:, 640:768]
        self.blkb = self.cstb[:, 512:640]
        self.blkf = self.cst[:, 512:640]
        self.eps_rms = self.cst[:, 1344:1345]
        self.eps_gn = self.cst[:, 1345:1346]

    def load_x(self, src):
        self.wreset()
        xin = [self.walloc(D, F32) for _ in range(2)]
        for tt in range(16):
            b = xin[tt % 2]
            self.dma("sp", b, src[tt * 128:(tt + 1) * 128, :], [], [("xin", tt % 2)])
            for half in range(2):
                bk = 6 + half
                for j in range(4):
                    c = half * 4 + j
                    self.tr(self.bank(bk, j * 128, (j + 1) * 128), b[:, c * 128:(c + 1) * 128], self.ident,
                            [("xin", tt % 2), "cst"], [("ps", bk)])
                o = self.xT[:, half * 4:(half + 1) * 4, tt * 128:(tt + 1) * 128]
                i = self.bank(bk).rearrange("p (c t) -> p c t", c=4)
                self.cp("dve" if half == 0 else "act", o, i, [("ps", bk)], [("xT", tt // 4)])

    def store_x(self, dst):
        self.wreset()
        xo = [self.walloc(D, F32) for _ in range(2)]
        for tt in range(16):
            b = xo[tt % 2]
            for half in range(2):
                bk = 6 + half
                for j in range(4):
                    c = half * 4 + j
                    self.tr(self.bank(bk, j * 128, (j + 1) * 128), self.xT[:, c, tt * 128:(tt + 1) * 128], self.ident,
                            [("xT", tt // 4), "cst"], [("ps", bk)])
                self.cp("dve" if half == 0 else "act", b[:, half * 512:(half + 1) * 512], self.bank(bk),
                        [("ps", bk)], [("xo", tt % 2)])
            self.dma("sp", dst[tt * 128:(tt + 1) * 128, :], b, [("xo", tt % 2)], [])

    def prenorm(self, gvec, tmp_off):
        self.wreset(tmp_off)
        sq = self.walloc(8 * 512, BF16).rearrange("p (c t) -> p c t", c=8)
        t1 = self.walloc(512, F32)
        rstd = self.walloc(512, F32)
        for c in range(8):
            self.memset("pool", self.hT[:, c, 0:1], 0.0, [("hT", 0)])
        for tg in range(4):
            sl = slice(tg * 512, (tg + 1) * 512)
            for c in range(8):
                self.act(sq[:, c, :], self.xT[:, c, sl], AF.Square, [("xT", tg)], [("pn_sq", c)])
                self.mm(self.bank(7), self.onesb, sq[:, c, :], c == 0, c == 7, [("pn_sq", c), "cstb"], [("ps", 7)])
            self.act(t1, self.bank(7), AF.Ln, [("ps", 7), "cst"], ["pn_t1"], bias=self.eps_rms, scale=1.0 / D)
            self.act(rstd, t1, AF.Exp, ["pn_t1"], ["pn_rstd"], scale=-0.5)
            for c in range(8):
                self.stt(self.hT[:, c, 1 + tg * 512: 1 + (tg + 1) * 512], self.xT[:, c, sl], self.pcol(gvec, c), rstd,
                         ALU.mult, ALU.mult, [("xT", tg), "pn_rstd", "par"], [("hT", tg)])

    def postnorm_alloc(self, off):
        self.wreset(off)
        self.pn_o = self.walloc(8 * 512, F32).rearrange("p (c t) -> p c t", c=8)
        self.pn_sq = self.walloc(8 * 512, BF16).rearrange("p (c t) -> p c t", c=8)
        self.pn_t = self.walloc(512, F32)
        self.pn_r = self.walloc(512, F32)
        self.pn_u = self.walloc(512, F32)

    def postnorm_evac(self, bk, oc):
        self.cp("dve", self.pn_o[:, oc, :], self.bank(bk), [("ps", bk)], [("po_o", oc)])
        self.act(self.pn_sq[:, oc, :], self.pn_o[:, oc, :], AF.Square, [("po_o", oc)], [("po_sq", oc)])

    def postnorm_finish(self, gvec, tg, statbk=7):
        sl = slice(tg * 512, (tg + 1) * 512)
        for oc in range(8):
            self.mm(self.bank(statbk), self.onesb, self.pn_sq[:, oc, :], oc == 0, oc == 7, [("po_sq", oc), "cstb"], [("ps", statbk)])
        self.act(self.pn_t, self.bank(statbk), AF.Ln, [("ps", statbk), "cst"], ["po_t"], bias=self.eps_rms, scale=1.0 / D)
        self.act(self.pn_r, self.pn_t, AF.Exp, ["po_t"], ["po_r"], scale=-0.5)
        for oc in range(8):
            self.stt(self.pn_o[:, oc, :], self.pn_o[:, oc, :], self.pcol(gvec, oc), self.pn_r, ALU.mult, ALU.mult,
                     [("po_o", oc), "po_r", "par"], [("po_o", oc)])
            self.tt("pool", self.xT[:, oc, sl], self.xT[:, oc, sl], self.pn_o[:, oc, :], ALU.add,
                    [("po_o", oc), ("xT", tg)], [("xT", tg)])

    def wload(self, dst3, src2, K, key):
        self.dma("pool", dst3, src2.rearrange("(k p) f -> p k f", p=128), [], [key])

    def rwkv(self):
        d, S = self.d, self.S
        self.wreset(OFF_X)
        LIM = OFF_H
        A = lambda n, dt: self.walloc(n, dt, LIM)
        tanhT = A(2048, BF16)
        a1T = A(2048, BF16)
        sgT = A(2 * 2048, BF16).rearrange("p (c t) -> p c t", c=2)
        wd2 = A(1024, BF16)
        a2w = A(1024, BF16)
        g2w = A(2 * 1024, BF16).rearrange("p (c f) -> p c f", c=2)
        self.dma("pool", wd2[0:64, :], d["w_decay2"][:, :], [], ["wd2"])
        self.dma("pool", a2w[0:64, :], d["a2"][:, :], [], ["a2w"])
        self.dma("pool", g2w[:, 0, :], d["g2"][0:128, :], [], ["g2w"])
        self.dma("pool", g2w[0:32, 1, :], d["g2"][128:160, :], [], ["g2w"])
        wraw = [A(8 * 128, BF16).rearrange("p (k f) -> p k f", k=8) for _ in range(1)]
        wab = [[A(8 * 128, BF16).rearrange("p (k f) -> p k f", k=8) for _ in range(2)] for _ in range(3)]
        pc = A(16, F32)
        S_all = A(17 * 64, F32).rearrange("p (n i) -> p n i", n=17)
        S_bf = A(4 * 64, BF16).rearrange("p (n i) -> p n i", n=4)
        Mabr = [A(512, BF16).rearrange("p (h w t) -> p h w t", h=2, w=2) for _ in range(4)]
        Makr = [A(512, BF16).rearrange("p (h w t) -> p h w t", h=2, w=2) for _ in range(4)]
        MabT = [A(256, BF16).rearrange("p (h t) -> p h t", h=2) for _ in range(4)]
        Tm = [[A(256, BF16).rearrange("p (h t) -> p h t", h=2) for _ in range(2)] for _ in range(4)]
        W1 = [A(128, BF16).rearrange("p (h i) -> p h i", h=2) for _ in range(4)]
        GT = A(4 * 64, F32).rearrange("p (n j) -> p n j", n=4)
        Hs = A(4 * 64, F32).rearrange("p (n j) -> p n j", n=4)
        ReffT = A(512, BF16)
        YV = A(512, F32)
        self.wreset(OFF_W)
        LIM = ASZ
        PP = [[A(512, BF16).rearrange("p (h w t) -> p h w t", h=2, w=2) for _ in range(2)] for _ in range(4)]
        AU = [A(256, BF16).rearrange("p (h w i) -> p h w i", h=2, w=2) for _ in range(4)]
        tmp = [A(512, F32) for _ in range(11)]
        kksq = A(512, BF16)
        S1O = []
        for _p in range(2):
            o = dict(ar=A(4 * 256, BF16).rearrange("p (n w t) -> p n w t", n=4, w=2))
            for nm in ("bt", "kt", "bh", "kh", "vfm", "rkr", "gfm"):
                o[nm] = A(512, BF16)
            o["tok"] = A(4 * 4 * 128, BF16).rearrange("p (n w f) -> p n w f", n=4, w=4)
            S1O.append(o)
        ybuf = A(512, F32)
        gt = [A(512, F32) for _ in range(4)]

        hT = self.hT
        mu_idx = {"r": 0, "w": 1, "k": 2, "v": 3, "a": 4, "g": 5}

        def scaled(dst_a, dst_b, raw, which, ncol, key_raw, key_w):
            i = mu_idx[which]
            mu_bc = self.par[:, 64 + i * 8: 72 + i * 8].unsqueeze(2).to_broadcast([128, 8, ncol])
            om_bc = self.par[:, NPAR + i * 8: NPAR + i * 8 + 8].unsqueeze(2).to_broadcast([128, 8, ncol])
            self.tt("pool", dst_a, raw, om_bc, ALU.mult, [key_raw, "par2"], [(key_w, 0)])
            self.tt("pool", dst_b, raw, mu_bc, ALU.mult, [key_raw, "par"], [(key_w, 1)])

        def proj(out_ps, wa, wb, M, tg, keyw, bk, mlo=0):
            for k in range(8):
                self.mm(out_ps, wa[:, k, mlo:mlo + M], hT[:, k, 1 + tg * 512: 1 + (tg + 1) * 512], k == 0, False,
                        [(keyw, 0), ("hT", tg)], [("ps", bk)])
            for k in range(8):
                self.mm(out_ps, wb[:, k, mlo:mlo + M], hT[:, k, tg * 512: (tg + 1) * 512], False, k == 7,
                        [(keyw, 1), ("hT", tg), ("hT", max(tg - 1, 0)), ("hT", 0)], [("ps", bk)])

        l1a = tmp[0].bitcast(BF16)[:, 0:1024]
        lw_raw = self.bf(OFF_W, 8 * 160).rearrange("p (k f) -> p k f", k=8)
        lw_a = self.bf(OFF_W + 4096, 8 * 160).rearrange("p (k f) -> p k f", k=8)
        lw_b = self.bf(OFF_W + 8192, 8 * 160).rearrange("p (k f) -> p k f", k=8)
        for which, src, ncol in (("w", d["w_decay1"], 64), ("a", d["a1"], 64), ("g", d["g1"], 160)):
            self.wload(lw_raw[:, :, 0:ncol], src[:, :], 8, "lw_raw")
            scaled(lw_a[:, :, 0:ncol], lw_b[:, :, 0:ncol], lw_raw[:, :, 0:ncol], which, ncol, "lw_raw", "lw")
            for tg in range(4):
                sl = slice(tg * 512, (tg + 1) * 512)
                if which == "w":
                    proj(self.bank(0)[0:64, :], lw_a, lw_b, 64, tg, "lw", 0)
                    self.act(tanhT[0:64, sl], self.bank(0)[0:64, :], AF.Tanh, [("ps", 0)], ["tanhT"])
                elif which == "a":
                    proj(self.bank(1)[0:64, :], lw_a, lw_b, 64, tg, "lw", 1)
                    self.cp("dve", a1T[0:64, sl], self.bank(1)[0:64, :], [("ps", 1)], ["a1T"])
                else:
                    proj(self.bank(2), lw_a, lw_b, 128, tg, "lw", 2)
                    self.act(sgT[:, 0, sl], self.bank(2), AF.Sigmoid, [("ps", 2)], ["sgT"])
                    proj(self.bank(3)[0:32, :], lw_a, lw_b, 32, tg, "lw", 3, mlo=128)
                    self.act(sgT[0:32, 1, sl], self.bank(3)[0:32, :], AF.Sigmoid, [("ps", 3)], ["sgT"])
        S.barrier()

        su_iu = self.cstb[:, 128:384].unsqueeze(1).to_broadcast([128, 2, 256])
        slm = self.cstb[:, 384:512].unsqueeze(1).to_broadcast([128, 2, 128])
        id2 = self.cstb[:, 0:128].unsqueeze(1).to_broadcast([128, 2, 128])
        identcol = self.cst[:, 1280:1344]

        def pair(b0, lo, hi):
            assert b0 % 2 == 0
            return self.pp[b0 // 2][:, :].rearrange("p (h x) -> p h x", h=2)[:, :, lo:hi]

        hs = [slice(0, 64), slice(64, 128)]
        NS = range(4)
        CS = [slice(n * 128, (n + 1) * 128) for n in NS]
        real_add = S.add

        def record(fn):
            rec = []
            S.add = lambda *a, **k: rec.append((a, k))
            try:
                fn()
            finally:
                S.add = real_add
            return rec

        def merge(L1, L2):
            n1, n2 = len(L1), len(L2)
            i = j = 0
            while i < n1 or j < n2:
                if j >= n2 or (i < n1 and i * n2 <= j * n1):
                    a, k = L1[i]; i += 1
                else:
                    a, k = L2[j]; j += 1
                real_add(*a, **k)

        def stage1(c, tg, p):
            O = S1O[p]
            ar, bt, kt, bh, kh, vfm, rkr, gfm, tok = (O[x] for x in ("ar", "bt", "kt", "bh", "kh", "vfm", "rkr", "gfm", "tok"))
            kp = lambda nm: (nm, p)
            sl = slice(tg * 512, (tg + 1) * 512)
            if tg == 0:
                for pi, which in enumerate(("r", "k", "v")):
                    raw = wraw[0]
                    self.wload(raw, d["w_rkv"][pi, :, c * 128:(c + 1) * 128], 8, ("wraw", 0))
                    scaled(wab[pi][0], wab[pi][1], raw, which, 128, ("wraw", 0), ("wab", pi))
                self.memset("pool", S_all[:, 0, :], 0.0, [("S_all", 0)])
            t = tmp
            K = lambda i: ("t", i)
            BW, BA, BK, BS, BR, BV, BG = 5, 6, 7, 5, 6, 7, 5
            self.mm(self.bank(BW), wd2[0:64, c * 128:(c + 1) * 128], tanhT[0:64, sl], True, True, ["wd2", "tanhT"], [("ps", BW)])
            self.act(t[0], self.bank(BW), AF.Sigmoid, [("ps", BW), "par"], [K(0)], bias=self.pcol(14, c))
            self.mm(self.bank(BA), a2w[0:64, c * 128:(c + 1) * 128], a1T[0:64, sl], True, True, ["a2w", "a1T"], [("ps", BA)])
            self.act(t[5], self.bank(BA), AF.Sigmoid, [("ps", BA), "par"], [K(5)], bias=self.pcol(15, c))
            proj(self.bank(BK), wab[1][0], wab[1][1], 128, tg, ("wab", 1), BK)
            k_ps = self.bank(BK)
            for n in range(4):
                cs = CS[n]
                S.add("dve", lambda e, o=t[1][:, cs], i0=t[0][:, cs], z=self.cst[:, 1346:1347].to_broadcast([128, 128]):
                      e.tensor_tensor_scan(o, i0, z, 0.0, ALU.add, ALU.add), reads=[K(0), "cst"], writes=[K(1)])
            self.tt("pool", t[0], t[1], t[0], ALU.subtract, [K(0), K(1)], [K(0)])
            self.act(t[2], t[1], AF.Exp, [K(1)], [K(2)], scale=C0)
            self.act(t[3], t[1], AF.Exp, [K(1)], [K(3)], scale=-C0)
            self.act(t[0], t[0], AF.Exp, [K(0)], [K(0)], scale=-C0)
            self.cp("pool", pc[:, tg * 4:(tg + 1) * 4], t[3].rearrange("p (n t) -> p n t", n=4)[:, :, 127], [K(3)], [("pc", tg)])
            self.tt("dve", t[4].rearrange("p (n t) -> p n t", n=4), t[2].rearrange("p (n t) -> p n t", n=4),
                    pc[:, tg * 4:(tg + 1) * 4].unsqueeze(2).to_broadcast([128, 4, 128]), ALU.mult, [K(2), ("pc", tg)], [K(4)])
            self.act(t[6], k_ps, AF.Identity, [("ps", BK), "par"], [K(6)], scale=self.pcol(16, c))
            self.ts("dve", t[8], t[5], self.pcol(17, c), self.par[:, NPAR + 48 + c:NPAR + 49 + c], ALU.mult, ALU.add, [K(5), "par", "par2"], [K(8)])
            self.tt("dve", t[8], k_ps, t[8], ALU.mult, [("ps", BK), K(8)], [K(8)])
            self.act(kksq, t[6], AF.Square, [K(6)], ["kksq"])
            self.mm(self.bank(BS), self.blkb, kksq, True, True, ["kksq", "cstb"], [("ps", BS)])
            proj(self.bank(BR), wab[0][0], wab[0][1], 128, tg, ("wab", 0), BR)
            proj(self.bank(BV), wab[2][0], wab[2][1], 128, tg, ("wab", 2), BV)
            r_ps, v_ps = self.bank(BR), self.bank(BV)
            self.ts("dve", t[7], self.bank(BS), 1e-19, None, ALU.max, None, [("ps", BS)], [K(7)])
            self.act(t[7], t[7], AF.Ln, [K(7)], [K(7)])
            self.act(t[7], t[7], AF.Exp, [K(7)], [K(7)], scale=-0.5)
            self.tt("pool", t[6], t[6], t[7], ALU.mult, [K(6), K(7)], [K(6)])
            self.mm(self.bank(BG), g2w[:, 0, c * 128:(c + 1) * 128], sgT[:, 0, sl], True, False, ["g2w", "sgT"], [("ps", BG)])
            self.mm(self.bank(BG), g2w[0:32, 1, c * 128:(c + 1) * 128], sgT[0:32, 1, sl], False, True, ["g2w", "sgT"], [("ps", BG)])
            self.stt(ar[:, :, 0, :], t[6].rearrange("p (n t) -> p n t", n=4), -1.0, t[0].rearrange("p (n t) -> p n t", n=4),
                     ALU.mult, ALU.mult, [K(6), K(0)], [kp("ar_a")])
            self.tt("pool", t[6], t[6], t[5], ALU.mult, [K(6), K(5)], [K(6)])
            self.tt("dve", bt, t[6], t[2], ALU.mult, [K(6), K(2)], [kp("bt")])
            self.tt("pool", bh, t[6], t[4], ALU.mult, [K(6), K(4)], [kp("bh")])
            self.tt("pool", kt, t[8], t[2], ALU.mult, [K(8), K(2)], [kp("kt")])
            self.tt("pool", kh, t[8], t[4], ALU.mult, [K(8), K(4)], [kp("kh")])
            self.tt("dve", ar[:, :, 1, :], r_ps.rearrange("p (n t) -> p n t", n=4), t[3].rearrange("p (n t) -> p n t", n=4),
                    ALU.mult, [("ps", BR), K(3)], [kp("ar_r")])
            self.stt(rkr, r_ps, self.pcol(18, c), t[8], ALU.mult, ALU.mult, [("ps", BR), K(8), "par"], [kp("rkr")])
            self.cp("act", vfm, v_ps, [("ps", BV)], [kp("vfm")])
            self.cp("act", gfm, self.bank(BG), [("ps", BG)], [kp("gfm")])
            for n in range(4):
                cs = CS[n]
                tb = (6, 7, 5, 6)[n]
                srcs = [(vfm[:, cs], kp("vfm")), (ar[:, n, 0, :], kp("ar_a")), (bh[:, cs], kp("bh")), (kh[:, cs], kp("kh"))]
                for w, (src, key) in enumerate(srcs):
                    self.tr(self.bankb(tb, w * 128, (w + 1) * 128), src, self.identb, [key, "cstb"], [("ps", tb)])
                self.cp("act", tok[:, n, :, :], self.bankb(tb, 0, 512).rearrange("p (w f) -> p w f", w=4), [("ps", tb)], [("tok", p, n)])

        def stage2(c, tg, p):
            O = S1O[p]
            ar, bt, kt, bh, kh, vfm, rkr, gfm, tok = (O[x] for x in ("ar", "bt", "kt", "bh", "kh", "vfm", "rkr", "gfm", "tok"))
            kp = lambda nm: (nm, p)
            sl = slice(tg * 512, (tg + 1) * 512)
            for rnd in range(2):
                for n in (2 * rnd, 2 * rnd + 1):
                    o = (n % 2) * 256
                    for h in range(2):
                        rhs = ar[hs[h], n, :, :]
                        self.mm(self.bank(h, o, o + 256), bt[hs[h], CS[n]], rhs, True, True, [kp("bt"), kp("ar_a"), kp("ar_r")], [("ps", h)])
                        self.mm(self.bank(2 + h, o, o + 256), kt[hs[h], CS[n]], rhs, True, True, [kp("kt"), kp("ar_a"), kp("ar_r")], [("ps", 2 + h)])
                for n in (2 * rnd, 2 * rnd + 1):
                    o = (n % 2) * 256
                    self.tt("dve", Mabr[n].rearrange("p h w t -> p h (w t)"), pair(0, o, o + 256), su_iu, ALU.mult,
                            [("ps", 0), ("ps", 1), "cstb"], [("Mabr", n)])
                    self.tt("dve", Makr[n].rearrange("p h w t -> p h (w t)"), pair(2, o, o + 256), su_iu, ALU.mult,
                            [("ps", 2), ("ps", 3), "cstb"], [("Makr", n)])
            for n in NS:
                for h in range(2):
                    self.mm(self.bank(h, n * 128, (n + 1) * 128), ar[hs[h], n, 0, :], bt[hs[h], CS[n]], True, True,
                            [kp("bt"), kp("ar_a")], [("ps", h)])
            for n in NS:
                self.tt("dve", MabT[n], pair(0, n * 128, (n + 1) * 128), slm, ALU.mult, [("ps", 0), ("ps", 1), "cstb"], [("MabT", n)])
            cur = 0
            for n in NS:
                self.tt("pool", Tm[n][cur], Mabr[n][:, :, 0, :], id2, ALU.add, [("Mabr", n), "cstb"], [("Tm", n, cur)])
            Pk = [lambda h, n=n: Mabr[n][:, h, 0, :] for n in NS]
            PkT = [lambda h, n=n: MabT[n][:, h, :] for n in NS]
            Rk = [[("Mabr", n), ("MabT", n)] for n in NS]
            def squares(s_, Pk, PkT, Rk):
                for n in NS:
                    for h in range(2):
                        o = h * 256
                        if s_ < 6:
                            self.mm(self.bank(n, o, o + 128), PkT[n](h), Pk[n](h), True, True, Rk[n], [("ps", n)])
                        self.mm(self.bank(n, o + 128, o + 256), Pk[n](h), PkT[n](h), True, True, Rk[n], [("ps", n)])

            def evacpp(s_):
                pq = s_ % 2
                for n in NS:
                    self.cp("act", PP[n][pq].rearrange("p h w t -> p (h w t)"), self.bank(n), [("ps", n)], [("PP", n, pq)])
                return ([lambda h, n=n, pq=pq: PP[n][pq][:, h, 0, :] for n in NS],
                        [lambda h, n=n, pq=pq: PP[n][pq][:, h, 1, :] for n in NS],
                        [[("PP", n, pq)] for n in NS])

            squares(1, Pk, PkT, Rk)
            Pk, PkT, Rk = evacpp(1)
            for s_ in range(1, 7):
                pq = s_ % 2
                if s_ < 6:
                    squares(s_ + 1, Pk, PkT, Rk)
                for rnd in range(2):
                    for n in (2 * rnd, 2 * rnd + 1):
                        for h in range(2):
                            o = ((n % 2) * 2 + h) * 128
                            self.mm(self.bank(4, o, o + 128), PkT[n](h), Tm[n][cur][:, h, :], True, True,
                                    [("PP", n, pq), ("Tm", n, cur)], [("ps", 4)])
                    for n in (2 * rnd, 2 * rnd + 1):
                        o = (n % 2) * 256
                        self.tt("dve", Tm[n][1 - cur].rearrange("p h t -> p (h t)"), self.bank(4, o, o + 256),
                                Tm[n][cur].rearrange("p h t -> p (h t)"), ALU.add, [("ps", 4), ("Tm", n, cur)], [("Tm", n, 1 - cur)])
                cur = 1 - cur
                if s_ < 6:
                    Pk, PkT, Rk = evacpp(s_ + 1)
            for n in NS:
                for h in range(2):
                    o = (n * 2 + h) * 64
                    self.mm(self.bank(0, o, o + 64), Makr[n][:, h, 0, :], tok[:, n, 0, hs[h]], True, True, [("Makr", n), ("tok", p, n)], [("ps", 0)])
            for n in NS:
                self.cp("act", W1[n].rearrange("p h i -> p (h i)"), self.bank(0, n * 128, (n + 1) * 128), [("ps", 0)], [("W1", n)])
            for n in NS:
                TmF, kT_ = Tm[n][cur], ("Tm", n, cur)
                bb = 1 + n // 2
                for h in range(2):
                    o = ((n % 2) * 2 + h) * 128
                    self.mm(self.bank(bb, o, o + 64), TmF[:, h, :], tok[:, n, 1, hs[h]], True, True, [kT_, ("tok", p, n)], [("ps", bb)])
                    self.mm(self.bank(bb, o + 64, o + 128), TmF[:, h, :], W1[n][:, h, :], True, True, [kT_, ("W1", n)], [("ps", bb)])
            for n in NS:
                bb = 1 + n // 2
                o = (n % 2) * 256
                self.cp("act", AU[n].rearrange("p h w i -> p (h w i)"), self.bank(bb, o, o + 256), [("ps", bb)], [("AU", n)])
            for n in NS:
                for h in range(2):
                    Ae, UV = AU[n][:, h, 0, :], AU[n][:, h, 1, :]
                    ka = ("AU", n)
                    tk = ("tok", p, n)
                    self.mm(self.bank(3, n * 128, (n + 1) * 128)[hs[h], :], Ae, Mabr[n][:, h, 1, :], True, True, [ka, ("Mabr", n)], [("ps", 3)])
                    self.mm(self.bank(4, n * 64, (n + 1) * 64)[hs[h], :], Ae, tok[:, n, 2, hs[h]], True, True, [ka, tk], [("ps", 4)])
                    self.mm(self.bank(4, 256 + n * 64, 256 + (n + 1) * 64)[hs[h], :], tok[:, n, 2, hs[h]], UV, True, False, [ka, tk], [("ps", 4)])
                    self.mm(self.bank(4, 256 + n * 64, 256 + (n + 1) * 64)[hs[h], :], tok[:, n, 3, hs[h]], tok[:, n, 0, hs[h]], False, True, [tk], [("ps", 4)])
                    self.mm(self.bank(0, n * 128, (n + 1) * 128)[hs[h], :], UV, Mabr[n][:, h, 1, :], True, False, [ka, ("Mabr", n)], [("ps", 0)])
                    self.mm(self.bank(0, n * 128, (n + 1) * 128)[hs[h], :], tok[:, n, 0, hs[h]], Makr[n][:, h, 1, :], False, True, [tk, ("Makr", n)], [("ps", 0)])
            self.tt("dve", ReffT, self.bank(3), ar[:, :, 1, :], ALU.add, [("ps", 3), kp("ar_r")], [("ReffT", n) for n in NS])
            for n in NS:
                gn = tg * 4 + n
                self.stt(GT[:, n, :], identcol, pc[:, gn:gn + 1], self.bank(4, n * 64, (n + 1) * 64), ALU.mult, ALU.add,
                         [("ps", 4), ("pc", tg), "cst"], [("GT", n)])
            self.cp("act", Hs.rearrange("p n j -> p (n j)"), self.bank(4, 256, 512), [("ps", 4)], [("Hs", n) for n in NS])
            self.cp("act", YV, self.bank(0), [("ps", 0)], [("YV", n) for n in NS])
            for n in NS:
                gn = tg * 4 + n
                for h in range(2):
                    ob_ = self.bank(1 + h, n * 64, (n + 1) * 64)[hs[h], :]
                    self.mm(ob_, GT[hs[h], n, :], S_all[hs[h], gn, :], True, True, [("GT", n), ("S_all", gn)], [("ps", 1 + h)])
                    self.tt("dve", S_all[hs[h], gn + 1, :], ob_, Hs[hs[h], n, :], ALU.add,
                            [("ps", 1 + h), ("Hs", n)], [("S_all", gn + 1)])
                self.cp("pool", S_bf[:, n, :], S_all[:, gn, :], [("S_all", gn)], [("S_bf", n)])
            for n in NS:
                for h in range(2):
                    self.mm(self.bank(3 + h)[hs[h], CS[n]], S_bf[hs[h], n, :], ReffT[hs[h], CS[n]], True, True,
                            [("S_bf", n), ("ReffT", n)], [("ps", 3 + h)])
            for h in range(2):
                self.tt("dve", ybuf[hs[h], :], self.bank(3 + h)[hs[h], :], YV[hs[h], :], ALU.add,
                        [("ps", 3 + h)] + [("YV", n) for n in NS], [("y", h)])
            Ky = [("y", 0), ("y", 1)]
            self.tt("dve", gt[0], ybuf, ybuf, ALU.mult, Ky, ["g0"])
            self.mm(self.bank(0), self.blkf, ybuf, True, True, Ky + ["cst"], [("ps", 0)])
            self.mm(self.bank(1), self.blkf, gt[0], True, True, ["g0", "cst"], [("ps", 1)])
            self.mm(self.bank(2), self.blkb, rkr, True, True, [kp("rkr"), "cstb"], [("ps", 2)])
            self.act(gt[1], self.bank(0), AF.Copy, [("ps", 0)], ["g1"], scale=1.0 / 64)
            self.tt("dve", gt[2], gt[1], gt[1], ALU.mult, ["g1"], ["g2"])
            self.stt(gt[2], self.bank(1), 1.0 / 64, gt[2], ALU.mult, ALU.subtract, [("ps", 1), "g2"], ["g2"])
            self.act(gt[2], gt[2], AF.Ln, ["g2", "cst"], ["g2"], bias=self.eps_gn)
            self.act(gt[2], gt[2], AF.Exp, ["g2"], ["g2"], scale=-0.5)
            self.tt("dve", gt[0], ybuf, gt[1], ALU.subtract, Ky + ["g1"], ["g0"])
            self.tt("dve", gt[0], gt[0], gt[2], ALU.mult, ["g0", "g2"], ["g0"])
            self.ts("dve", gt[0], gt[0], self.pcol(19, c), self.pcol(20, c), ALU.mult, ALU.add, ["g0", "par"], ["g0"])
            self.tt("dve", gt[3], self.bank(2), vfm, ALU.mult, [("ps", 2), kp("vfm")], ["g3"])
            self.tt("dve", gt[0], gt[0], gt[3], ALU.add, ["g0", "g3"], ["g0"])
            self.tt("dve", self.zT[:, c, sl], gt[0], gfm, ALU.mult, ["g0", kp("gfm")], [("zT", tg)])

        seq = [(c, tg) for c in range(8) for tg in range(4)]
        stage1(*seq[0], 0)
        for k in range(len(seq)):
            L1 = record(lambda: stage1(*seq[k + 1], (k + 1) % 2)) if k + 1 < len(seq) else []
            L2 = record(lambda: stage2(*seq[k], k % 2))
            merge(L1, L2)

    def rwkv_out(self, x_src):
        d = self.d
        self.S.barrier()
        self.load_x(x_src)
        self.wreset(OFF_W + 8192)
        wo = self.walloc(8 * 1024, BF16).rearrange("p (k f) -> p k f", k=8)
        self.wload(wo, d["w_o_rwkv"][:, :], 8, "wo")
        self.postnorm_alloc(self.wtop)
        for tg in range(4):
            sl = slice(tg * 512, (tg + 1) * 512)
            for oc in range(8):
                bk = oc % 4
                for k in range(8):
                    self.mm(self.bank(bk), wo[:, k, oc * 128:(oc + 1) * 128], self.zT[:, k, sl], k == 0, k == 7,
                            ["wo", ("zT", tg)], [("ps", bk)])
                self.postnorm_evac(bk, oc)
            self.postnorm_finish(1, tg)

    def ffn(self, l):
        d, S = self.d, self.S
        S.barrier()
        self.prenorm(4 * l + 2, OFF_W)
        S.barrier()
        G = self.bf(OFF_Z, 22 * 1024).rearrange("p (k t) -> p k t", k=22)
        FW = OFF_Z + 22 * 1024 * 2
        self.wreset(FW)
        halo = self.walloc(44 * 2, F32).rearrange("p (c t) -> p c t", c=44)
        wdn = [self.walloc(22 * 128, BF16).rearrange("p (k f) -> p k f", k=22) for _ in range(2)]
        TOP = self.wtop
        cw = lambda k, ch: self.par[:, 176 + (l * 3 + k) * 44 + ch: 176 + (l * 3 + k) * 44 + ch + 1]
        cb = lambda ch: self.par[:, 440 + l * 44 + ch: 440 + l * 44 + ch + 1]
        hT = self.hT
        NW = 8
        PF = 5
        for hf in range(2):
            self.wreset(TOP)
            wup = [self.walloc(8 * 128, BF16).rearrange("p (k f) -> p k f", k=8) for _ in range(NW)]
            cv = [self.walloc(1024, F32) for _ in range(4)]
            ga = [self.walloc(1024, F32) for _ in range(2)]
            units = [(i, w) for i in range(22) for w in range(2)]

            def issue_w(n):
                i, w = units[n]
                ch = w * 22 + i
                self.wload(wup[n % NW], d["w_up"][l, :, ch * 128:(ch + 1) * 128], 8, ("wup", n % NW))

            for n in range(min(PF, len(units))):
                issue_w(n)
            for n, (i, w) in enumerate(units):
                if n + PF < len(units):
                    issue_w(n + PF)
                ch = w * 22 + i
                wt, kw = wup[n % NW], ("wup", n % NW)
                b0 = (n % 4) * 2
                for g in range(2):
                    tg = hf * 2 + g
                    for k in range(8):
                        self.mm(self.bank(b0 + g), wt[:, k, :], hT[:, k, 1 + tg * 512: 1 + (tg + 1) * 512], k == 0, k == 7,
                                [kw, ("hT", tg)], [("ps", b0 + g)])
                ps = self.pp[b0 // 2][:, 0:1024]
                PK = [("ps", b0), ("ps", b0 + 1)]
                Cv, kc = cv[n % 4], ("cv", n % 4)
                self.act(Cv, ps, AF.Identity, PK + ["par"], [kc], bias=cb(ch), scale=cw(2, ch))
                self.stt(Cv[:, 1:1024], ps[:, 0:1023], cw(1, ch), Cv[:, 1:1024], ALU.mult, ALU.add, PK + [kc, "par"], [kc])
                self.stt(Cv[:, 2:1024], ps[:, 0:1022], cw(0, ch), Cv[:, 2:1024], ALU.mult, ALU.add, PK + [kc, "par"], [kc])
                if hf == 0:
                    self.cp("dve", halo[:, ch, :], ps[:, 1022:1024], PK, [("halo", ch)])
                else:
                    self.stt(Cv[:, 0:2], halo[:, ch, :], cw(0, ch), Cv[:, 0:2], ALU.mult, ALU.add, [("halo", ch), kc, "par"], [kc])
                    self.stt(Cv[:, 0:1], halo[:, ch, 1:2], cw(1, ch), Cv[:, 0:1], ALU.mult, ALU.add, [("halo", ch), kc, "par"], [kc])
                if w == 1:
                    ca, cu = cv[(n - 1) % 4], cv[n % 4]
                    gt_, kg = ga[i % 2], ("ga", i % 2)
                    self.act(gt_, ca, AF.Gelu_apprx_tanh, [("cv", (n - 1) % 4)], [kg])
                    self.tt("pool", G[:, i, :], gt_, cu, ALU.mult, [kg, ("cv", n % 4)], [("G", i)])
            self.wload(wdn[0], d["w_down"][l, :, 0:128], 22, ("wdn", 0))
            S.barrier()
            self.wreset(TOP)
            o_sb = self.walloc(8 * 1024, F32).rearrange("p (c t) -> p c t", c=8)
            osq = [self.walloc(512, BF16) for _ in range(4)]
            pn_t = self.walloc(1024, F32)
            pn_r = self.walloc(1024, F32)
            pend = []

            def flush():
                for (oc_, g_, q_) in pend:
                    self.mm(self.bank(6 + g_), self.onesb, osq[q_], oc_ == 0, oc_ == 7, [("osq", q_), "cstb"], [("ps", 6 + g_)])
                pend.clear()

            for oc in range(8):
                if oc + 1 < 8:
                    self.wload(wdn[(oc + 1) % 2], d["w_down"][l, :, (oc + 1) * 128:(oc + 2) * 128], 22, ("wdn", (oc + 1) % 2))
                wd, kw = wdn[oc % 2], ("wdn", oc % 2)
                for g in range(2):
                    bk = (oc % 2) * 2 + g
                    for k in range(22):
                        self.mm(self.bank(bk), wd[:, k, :], G[:, k, g * 512:(g + 1) * 512], k == 0, k == 21,
                                [kw, ("G", k)], [("ps", bk)])
                flush()
                for g in range(2):
                    bk = (oc % 2) * 2 + g
                    q = (oc * 2 + g) % 4
                    self.cp("dve", o_sb[:, oc, g * 512:(g + 1) * 512], self.bank(bk), [("ps", bk)], [("o_sb", oc, g)])
                    self.act(osq[q], o_sb[:, oc, g * 512:(g + 1) * 512], AF.Square, [("o_sb", oc, g)], [("osq", q)])
                    pend.append((oc, g, q))
            flush()
            gvec = 4 * l + 3
            for g in range(2):
                tg = hf * 2 + g
                sl = slice(tg * 512, (tg + 1) * 512)
                gs = slice(g * 512, (g + 1) * 512)
                self.act(pn_t[:, gs], self.bank(6 + g), AF.Ln, [("ps", 6 + g), "cst"], [("pn_t", g)], bias=self.eps_rms, scale=1.0 / D)
                self.act(pn_r[:, gs], pn_t[:, gs], AF.Exp, [("pn_t", g)], [("pn_r", g)], scale=-0.5)
                for oc in range(8):
                    self.stt(o_sb[:, oc, gs], o_sb[:, oc, gs], self.pcol(gvec, oc), pn_r[:, gs], ALU.mult, ALU.mult,
                             [("o_sb", oc, g), ("pn_r", g), "par"], [("o_sb", oc, g)])
                    self.tt("pool", self.xT[:, oc, sl], self.xT[:, oc, sl], o_sb[:, oc, gs], ALU.add,
                            [("o_sb", oc, g), ("xT", tg)], [("xT", tg)])
            S.barrier()

    def kv(self):
        d, S = self.d, self.S
        S.barrier()
        self.prenorm(21, OFF_W)
        self.kT = self.bf(OFF_Z, 4 * T).rearrange("p (h t) -> p h t", h=4)
        self.vtok = self.bf(OFF_Z + 16384, 16 * 256).rearrange("p (b f) -> p b f", b=16)
        self.wreset(OFF_W + 16384)
        wkv = self.walloc(8 * 512, BF16).rearrange("p (k f) -> p k f", k=8)
        wkd = self.walloc(8 * 512, BF16).rearrange("p (k h e) -> p k h e", k=8, h=4)
        self.wload(wkv, d["w_kv"][:, :], 8, "wkv")
        kview = wkv[:, :, 0:256].rearrange("p k (h e) -> p k h e", h=4)
        self.cp("pool", wkd[:, :, :, 0:64], kview, ["wkv"], ["wkd"])
        self.cp("pool", wkd[:, :, :, 64:128], kview, ["wkv"], ["wkd"])
        hT = self.hT
        for kvh in range(4):
            for tg in range(4):
                bk = (kvh * 4 + tg) % 4
                for k in range(8):
                    self.mm(self.bank(bk), wkd[:, k, kvh, :], hT[:, k, 1 + tg * 512: 1 + (tg + 1) * 512], k == 0, k == 7,
                            ["wkd", ("hT", tg)], [("ps", bk)])
                self.cp("act" if tg % 2 else "dve", self.kT[:, kvh, tg * 512:(tg + 1) * 512], self.bank(bk), [("ps", bk)], ["kT"])
        for tb in range(16):
            bk = 4 + tb % 4
            for k in range(8):
                self.mm(self.bank(bk, 0, 256), hT[:, k, 1 + tb * 128: 1 + (tb + 1) * 128], wkv[:, k, 256:512], k == 0, k == 7,
                        ["wkv", ("hT", tb // 4)], [("ps", bk)])
            self.cp("act" if tb % 2 else "dve", self.vtok[:, tb, :], self.bank(bk, 0, 256), [("ps", bk)], ["vtok"])

    def attn(self):
        d, S = self.d, self.S
        S.barrier()
        self.prenorm(4, OFF_W)
        S.barrier()
        self.wreset(OFF_W)
        qT = self.walloc(8 * T, BF16).rearrange("p (c t) -> p c t", c=8)
        wo = self.walloc(8 * 1024, BF16).rearrange("p (k f) -> p k f", k=8)
        TOP = self.wtop
        wq = [self.walloc(8 * 128, BF16).rearrange("p (k f) -> p k f", k=8) for _ in range(2)]
        hT = self.hT
        self.wload(wo, d["w_o_attn"][:, :], 8, "wo")
        for oc in range(8):
            w = wq[oc % 2]
            self.wload(w, d["w_q"][:, oc * 128:(oc + 1) * 128], 8, ("wq", oc % 2))
            for tg in range(4):
                bk = (oc * 4 + tg) % 8
                for k in range(8):
                    self.mm(self.bank(bk), w[:, k, :], hT[:, k, 1 + tg * 512: 1 + (tg + 1) * 512], k == 0, k == 7,
                            [("wq", oc % 2), ("hT", tg)], [("ps", bk)])
                self.cp("act" if tg % 2 else "dve", qT[:, oc, tg * 512:(tg + 1) * 512], self.bank(bk), [("ps", bk)], [("qT", oc)])
        S.barrier()
        self.wreset(TOP)
        ND = 4
        ssb = [self.walloc(2 * 260, F32).rearrange("p (h k) -> p h k", h=2) for _ in range(ND)]
        Pb = [self.walloc(2 * 260, BF16).rearrange("p (h k) -> p h k", h=2) for _ in range(ND)]
        PT = [self.walloc(512, BF16).rearrange("p (h j q) -> p h j q", h=2, j=2) for _ in range(ND)]
        sm = [self.walloc(8, F32) for _ in range(ND)]
        otok = [self.walloc(1024, BF16) for _ in range(2)]
        oT = self.bf(OFF_Z + 24576, 8 * 512).rearrange("p (c t) -> p c t", c=8)
        self.postnorm_alloc(OFF_H)
        amask = self.cst[:, 768:1024].unsqueeze(1).to_broadcast([128, 2, 256])
        amask0 = self.cst[:, 1024:1280].unsqueeze(1).to_broadcast([128, 2, 256])
        sinks = self.par[:, 528:544]
        units = [(b, c) for b in range(16) for c in range(8)]
        NU = len(units)

        def ctx(i):
            b, c = units[i]
            u = i % ND
            return dict(b=b, c=c, kvh=c // 2, u=u, v2=i % 2, kb0=max(b - 1, 0), sb=ssb[u], pb=Pb[u], pt=PT[u], sm=sm[u], ob=otok[b % 2])

        def ppair(b0, lo, hi):
            return self.pp[b0 // 2][:, :].rearrange("p (h x) -> p h x", h=2)[:, :, lo:hi]

        def S0(i):
            x = ctx(i)
            b, c, u = x["b"], x["c"], x["u"]
            b0 = 2 * x["v2"]
            for hh in range(2):
                hsl = slice(hh * 64, hh * 64 + 64)
                self.mm(self.bank(b0 + hh, 0, 256), qT[hsl, c, b * 128:(b + 1) * 128], self.kT[hsl, x["kvh"], x["kb0"] * 128:(x["kb0"] + 2) * 128],
                        True, True, [("qT", c), "kT"], [("ps", b0 + hh)])
            self.cp("pool", x["sb"][:, :, 256], sinks[:, 2 * c:2 * c + 2], ["par"], [("ssbk", u)])
            self.stt(x["sb"][:, :, 0:256], ppair(b0, 0, 256), 0.125, amask0 if b == 0 else amask, ALU.mult, ALU.add,
                     [("ps", b0), ("ps", b0 + 1), "cst"], [("ssb", u)])
            self.S.add("dve", lambda e, o=x["sm"][:, 0:2], i_=x["sb"][:, :, 0:257]: e.tensor_reduce(o, i_, AX.X, ALU.max, negate=True),
                       reads=[("ssb", u), ("ssbk", u)], writes=[("sm", u)])

        def S1(i):
            x = ctx(i)
            u = x["u"]
            for hh in range(2):
                self.act(x["pb"][:, hh, 0:257], x["sb"][:, hh, 0:257], AF.Exp, [("ssb", u), ("ssbk", u), ("sm", u)], [("Pb", u), ("sm2", u)],
                         bias=x["sm"][:, hh:hh + 1], accum_out=x["sm"][:, 2 + hh:3 + hh])
            self.recip(x["sm"][:, 4:6], x["sm"][:, 2:4], [("sm2", u)], [("sm3", u)])

        def S1b(i):
            x = ctx(i)
            u = x["u"]
            bt_ = 4 + x["v2"]
            for hh in range(2):
                for j in range(2):
                    o = (hh * 2 + j) * 128
                    self.tr(self.bankb(bt_, o, o + 128), x["pb"][:, hh, j * 128:(j + 1) * 128], self.identb,
                            [("Pb", u), "cstb"], [("ps", bt_)])
            self.cp("act", x["pt"].rearrange("p h j q -> p (h j q)"), self.bankb(bt_, 0, 512), [("ps", bt_)], [("PT", u)])

        def S2(i):
            x = ctx(i)
            u, b, c, kvh = x["u"], x["b"], x["c"], x["kvh"]
            bo = 6 + x["v2"]
            for hh in range(2):
                for j in range(2):
                    self.mm(self.bank(bo, hh * 64, hh * 64 + 64), x["pt"][:, hh, j, :], self.vtok[:, x["kb0"] + j, kvh * 64:(kvh + 1) * 64],
                            j == 0, j == 1, [("PT", u), "vtok"], [("ps", bo)])
            self.tt("dve", x["ob"][:, c * 128:(c + 1) * 128].rearrange("p (h e) -> p h e", h=2),
                    self.bank(bo, 0, 128).rearrange("p (h e) -> p h e", h=2),
                    x["sm"][:, 4:6].unsqueeze(2).to_broadcast([128, 2, 64]), ALU.mult,
                    [("ps", bo), ("sm3", u)], [("otok", b % 2)])
            if c == 7:
                tail(b)

        def tail(b):
            ob = otok[b % 2]
            for half in range(2):
                bk = 4 + half
                for j in range(4):
                    cc = half * 4 + j
                    self.tr(self.bankb(bk, j * 128, (j + 1) * 128), ob[:, cc * 128:(cc + 1) * 128], self.identb,
                            [("otok", b % 2), "cstb"], [("ps", bk)])
                self.cp("act", oT[:, half * 4:(half + 1) * 4, (b % 4) * 128:(b % 4 + 1) * 128],
                        self.bankb(bk, 0, 512).rearrange("p (c t) -> p c t", c=4), [("ps", bk)], ["oT"])
            if b % 4 == 3:
                tg = b // 4
                for oc in range(8):
                    bk = oc % 4
                    for k in range(8):
                        self.mm(self.bank(bk), wo[:, k, oc * 128:(oc + 1) * 128], oT[:, k, :], k == 0, k == 7, ["wo", "oT"], [("ps", bk)])
                    self.postnorm_evac(bk, oc)
                self.postnorm_finish(5, tg, statbk=7)

        for step in range(NU + 3):
            if step < NU:
                S0(step)
            if 0 <= step - 3 < NU:
                S2(step - 3)
            if 0 <= step - 1 < NU:
                S1(step - 1)
            if 0 <= step - 2 < NU:
                S1b(step - 2)


def build(stages=("rwkv", "ffn0", "kv", "attn", "ffn1"), dbg=()):
    nc = bass.Bass("TRN2", target_bir_lowering=False)
    dram = {}

    def din(name, shape):
        dram[name] = nc.dram_tensor(name, list(shape), F32, kind="ExternalInput").ap()

    din("x", [T, D]); din("cst", [128, NCST]); din("par", [128, NPAR])
    din("w_rkv", [3, D, D]); din("w_decay1", [D, 64]); din("w_decay2", [64, D]); din("a1", [D, 64]); din("a2", [64, D])
    din("g1", [D, 160]); din("g2", [160, D]); din("w_o_rwkv", [D, D]); din("w_kv", [D, 512]); din("w_q", [D, D])
    din("w_o_attn", [D, D]); din("w_up", [2, D, 2 * DFF]); din("w_down", [2, DFF, D])
    y = nc.dram_tensor("y", [T, D], F32, kind="ExternalOutput").ap()
    with ExitStack() as es:
        kb = KB(nc, es, dram)
        kb.setup()
        kb.load_x(dram["x"])
        if "rwkv" in stages:
            kb.S.barrier()
            kb.prenorm(0, OFF_W)
            kb.S.barrier()
            kb.rwkv()
            kb.rwkv_out(dram["x"])
        if "t_wload" in stages:
            kb.S.barrier()
            kb.wreset(OFF_W)
            wt = kb.walloc(8 * 128, BF16).rearrange("p (k f) -> p k f", k=8)
            kb.wload(wt, dram["w_q"][:, 0:128], 8, "twl")
        if "t_prenorm" in stages:
            kb.S.barrier()
            kb.prenorm(0, OFF_W)
        if "t_pool" in stages:
            kb.S.barrier()
            kb.wreset(OFF_W)
            tt_ = kb.walloc(512, F32)
            kb.memset("pool", tt_, 1.0, ["tp"])
            kb.tt("pool", tt_, tt_, tt_, ALU.mult, ["tp"], ["tp"])
        if "ffn0" in stages:
            kb.ffn(0)
        if "kv" in stages:
            kb.kv()
        if "attn" in stages:
            kb.attn()
        if "ffn1" in stages:
            kb.ffn(1)
        kb.S.barrier()
        kb.store_x(y)
        kb.S.emit(nc, es)
    return nc


def make_consts():
    c = np.zeros((128, NCST), np.float32)
    i = np.arange(128)
    c[:, 0:128] = np.eye(128)
    c[:, 128:256] = (i[:, None] < i[None, :])
    c[:, 256:384] = (i[:, None] <= i[None, :])
    c[:, 384:512] = (i[:, None] > i[None, :])
    c[:, 512:640] = (i[:, None] // 64 == i[None, :] // 64)
    c[:, 640:768] = 1.0
    q = i[:, None]
    kk = np.arange(256)[None, :]
    valid = (kk > q) & (kk <= q + 128)
    c[:, 768:1024] = np.where(valid, 0.0, -1e30)
    valid0 = (kk <= q)
    c[:, 1024:1280] = np.where(valid0, 0.0, -1e30)
    c[:, 1280:1344] = (i[:, None] % 64 == np.arange(64)[None, :])
    c[:, 1344] = 1e-6
    c[:, 1345] = 64e-5
    c[:, 1346] = 0.0
    return c


def make_par(inp):
    p = np.zeros((128, NPAR), np.float32)

    def put(v, vec):
        p[:, v * 8:(v + 1) * 8] = np.asarray(vec, np.float32).reshape(8, 128).T

    ng = inp["norm_g"]
    for l in range(2):
        for j in range(4):
            put(4 * l + j, ng[l, j])
    for i in range(6):
        put(8 + i, inp["mu"][0, i])
    put(14, inp["w_decay0"][0]); put(15, inp["a0"][0]); put(16, inp["k_k"][0]); put(17, inp["k_a"][0])
    put(18, inp["r_k"][0].reshape(-1)); put(19, inp["gn_g"][0]); put(20, inp["gn_b"][0]); put(21, inp["kv_g"])
    for l in range(2):
        for k in range(3):
            p[:, 176 + (l * 3 + k) * 44: 176 + (l * 3 + k + 1) * 44] = inp["conv_w"][l, k].reshape(44, 128).T
        p[:, 440 + l * 44: 440 + (l + 1) * 44] = inp["conv_b"][l].reshape(44, 128).T
    p[:, 528:544] = np.broadcast_to(inp["sinks"][0][None, :], (128, 16))
    return p


_NC = None


def kernel(**inputs):
    global _NC
    inp = {k: np.asarray(v) for k, v in inputs.items()}
    if _NC is None:
        _NC = build()
    cst = make_consts()
    par = make_par(inp)
    shared = {
        "cst": cst, "par": par,
        "w_rkv": np.ascontiguousarray(inp["w_rkv"][0]), "w_decay1": np.ascontiguousarray(inp["w_decay1"][0]),
        "w_decay2": np.ascontiguousarray(inp["w_decay2"][0]), "a1": np.ascontiguousarray(inp["a1"][0]),
        "a2": np.ascontiguousarray(inp["a2"][0]), "g1": np.ascontiguousarray(inp["g1"][0]),
        "g2": np.ascontiguousarray(inp["g2"][0]), "w_o_rwkv": np.ascontiguousarray(inp["w_o_rwkv"][0]),
        "w_kv": np.ascontiguousarray(inp["w_kv"]), "w_q": np.ascontiguousarray(inp["w_q"][0]),
        "w_o_attn": np.ascontiguousarray(inp["w_o_attn"][0]), "w_up": np.ascontiguousarray(inp["w_up"]),
        "w_down": np.ascontiguousarray(inp["w_down"]),
    }
    x = inp["x"].astype(np.float32)
    in_maps = [dict(shared, x=np.ascontiguousarray(x[i])) for i in range(8)]
    res = run_bass_kernel_spmd(_NC, in_maps, core_ids=list(range(8)))
    return np.stack([res.results[i]["y"] for i in range(8)], axis=0).astype(np.float32)
```

```python
import numpy as np
import concourse.bass as bass
import concourse.mybir as mybir
from contextlib import ExitStack
from concourse.bass_utils import run_bass_kernel_spmd

F32 = mybir.dt.float32
BF16 = mybir.dt.bfloat16
AF = mybir.ActivationFunctionType
ALU = mybir.AluOpType
AX = mybir.AxisListType

T, D, DFF = 2048, 1024, 2816
NSLOT = 24
NCST = 1352
NPAR = 544
C0 = float(np.exp(-0.5))
import os
CUT = int(os.environ.get('KCUT', '99'))


class Op:
    __slots__ = ("eng", "fn", "deps", "sig", "sigval", "dma", "slot", "idx")


class Sched:
    def __init__(self):
        self.ops = []
        self.last_w = {}
        self.readers = {}
        self.ndma = 0
        self.ndma_sw = 0
        self.slot_last = [None] * NSLOT
        self.slot_uses = [0] * NSLOT
        self.last_eng = {}
        self.pending = {}

    def barrier(self):
        s = set(self.last_eng.values())
        for x in self.slot_last:
            if x is not None:
                s.add(x)
        for e in ["pe", "act", "dve", "pool", "sp"]:
            self.pending[e] = set(s) | self.pending.get(e, set())

    def add(self, eng, fn, reads=(), writes=(), dma=False):
        op = Op()
        op.eng, op.fn, op.dma, op.sig, op.sigval, op.slot = eng, fn, dma, False, None, None
        op.idx = len(self.ops)
        deps = set()
        for k in reads:
            w = self.last_w.get(k)
            if w is not None:
                deps.add(w)
            if isinstance(k, tuple) and k[0] == "ps":
                for r in self.readers.get(k, ()):
                    if self.ops[r].eng != eng:
                        deps.add(r)
        for k in writes:
            w = self.last_w.get(k)
            if w is not None:
                deps.add(w)
            for r in self.readers.get(k, ()):
                deps.add(r)
        for k in reads:
            self.readers.setdefault(k, []).append(op.idx)
        for k in writes:
            self.last_w[k] = op.idx
            self.readers[k] = []
        if eng in self.pending:
            deps |= self.pending.pop(eng)
        if dma:
            if eng == "pool":
                s = NSLOT // 2 + self.ndma_sw % (NSLOT // 2)
                self.ndma_sw += 1
            else:
                s = self.ndma % (NSLOT // 2)
                self.ndma += 1
            if self.slot_last[s] is not None:
                deps.add(self.slot_last[s])
            self.slot_last[s] = op.idx
            self.slot_uses[s] += 1
            op.slot = s
            op.sigval = 16 * self.slot_uses[s]
            op.sig = True
        else:
            self.last_eng[eng] = op.idx
        deps.discard(op.idx)
        op.deps = sorted(deps)
        self.ops.append(op)
        return op

    def emit(self, nc, es):
        ops = self.ops
        for o in ops:
            for d in o.deps:
                p = ops[d]
                if p.dma:
                    continue
                if p.eng == "pe" and o.eng == "pe" and not o.dma:
                    continue
                p.sig = True
        cnt = {}
        for o in ops:
            if o.dma or not o.sig:
                continue
            cnt[o.eng] = cnt.get(o.eng, 0) + 1
            o.sigval = cnt[o.eng]
        engs = ["pe", "act", "dve", "pool", "sp"]
        esem = {e: es.enter_context(nc.semaphore("s_" + e)) for e in engs if e != "sp"}
        dsem = [es.enter_context(nc.semaphore("d_%d" % i)) for i in range(NSLOT)]
        block = es.enter_context(nc.Block())
        streams = {e: [o for o in ops if o.eng == e] for e in engs}

        def run(e, engine):
            waited = {}
            for o in streams[e]:
                for d in o.deps:
                    p = ops[d]
                    if p.dma:
                        key, sem, val = ("d", p.slot), dsem[p.slot], p.sigval
                    else:
                        if p.eng == "pe" and e == "pe" and not o.dma:
                            continue
                        key, sem, val = ("e", p.eng), esem[p.eng], p.sigval
                    if waited.get(key, 0) >= val:
                        continue
                    waited[key] = val
                    engine.wait_ge(sem, val)
                ins = o.fn(engine)
                if o.dma:
                    ins.then_inc(dsem[o.slot], 16)
                elif o.sig:
                    ins.then_inc(esem[e], 1)
            if e == "sp":
                for s in range(NSLOT):
                    if self.slot_uses[s] and waited.get(("d", s), 0) < 16 * self.slot_uses[s]:
                        engine.wait_ge(dsem[s], 16 * self.slot_uses[s])

        @block.tensor
        def _(eng):
            run("pe", eng)

        @block.scalar
        def _(eng):
            run("act", eng)

        @block.vector
        def _(eng):
            run("dve", eng)

        @block.gpsimd
        def _(eng):
            run("pool", eng)

        @block.sync
        def _(eng):
            run("sp", eng)


OFF_X = 0
OFF_H = 65536
OFF_Z = 98432
OFF_W = 131200
ASZ = 202240


class KB:
    def __init__(self, nc, es, dram):
        self.nc, self.es, self.S, self.d = nc, es, Sched(), dram
        self.arena = es.enter_context(nc.sbuf_tensor("arena", [128, ASZ // 4], F32))
        self.cst = es.enter_context(nc.sbuf_tensor("cst_sb", [128, NCST], F32))
        self.cstb = es.enter_context(nc.sbuf_tensor("cstb_sb", [128, NCST], BF16))
        self.par = es.enter_context(nc.sbuf_tensor("par_sb", [128, NPAR + 56], F32))
        self.pp = [es.enter_context(nc.psum_tensor("pp%d" % i, [128, 1024], F32)) for i in range(4)]
        self.wtop = OFF_W
        self.xT = self.f32(OFF_X, 8 * T).rearrange("p (c t) -> p c t", c=8)
        self.hT = self.bf(OFF_H, 8 * 2049).rearrange("p (c t) -> p c t", c=8)
        self.zT = self.bf(OFF_Z, 8 * T).rearrange("p (c t) -> p c t", c=8)
        self.uid = 0

    def f32(self, off, n):
        assert off % 4 == 0 and off + 4 * n <= ASZ, (off, n)
        return self.arena[:, off // 4: off // 4 + n]

    def bf(self, off, n):
        assert off % 4 == 0 and n % 2 == 0 and off + 2 * n <= ASZ, (off, n)
        return self.arena[:, off // 4: off // 4 + n // 2].bitcast(BF16)

    def wreset(self, off=OFF_W):
        self.wtop = off

    def walloc(self, n, dt, lim=ASZ):
        sz = n * (4 if dt == F32 else 2)
        sz = (sz + 63) // 64 * 64
        off = self.wtop
        self.wtop += sz
        assert self.wtop <= lim, ("arena overflow", self.wtop, lim)
        return self.f32(off, n) if dt == F32 else self.bf(off, n)

    def bank(self, b, lo=0, hi=512):
        return self.pp[b // 2][:, (b % 2) * 512 + lo: (b % 2) * 512 + hi]

    def bankb(self, b, lo=0, hi=1024):
        v = self.pp[b // 2][:, (b % 2) * 512: (b % 2) * 512 + 512].bitcast(BF16)
        return v[:, lo:hi]

    def pcol(self, v, c=None, n=8):
        if c is None:
            return self.par[:, v * 8: v * 8 + n]
        return self.par[:, v * 8 + c: v * 8 + c + 1]

    def mm(self, out, lhsT, rhs, start, stop, R, W):
        self.S.add("pe", lambda e: e.matmul(out, lhsT, rhs, start=start, stop=stop), reads=R, writes=W)

    def tr(self, out, in_, ident, R, W):
        self.S.add("pe", lambda e: e.transpose(out, in_, ident), reads=R, writes=W)

    def act(self, out, in_, func, R, W, bias=None, scale=None, accum_out=None, eng="act"):
        kw = {}
        if bias is not None:
            kw["bias"] = bias
        if scale is not None:
            kw["scale"] = scale
        if accum_out is not None:
            kw["accum_out"] = accum_out
        self.S.add("act", lambda e: e.activation(out, in_, func, **kw), reads=R, writes=W)

    def tt(self, eng, out, in0, in1, op, R, W):
        self.S.add(eng, lambda e: e.tensor_tensor(out, in0, in1, op), reads=R, writes=W)

    def ts(self, eng, out, in0, s1, s2, op0, op1, R, W):
        if op1 is None:
            self.S.add(eng, lambda e: e.tensor_scalar(out, in0, s1, None, op0), reads=R, writes=W)
        else:
            self.S.add(eng, lambda e: e.tensor_scalar(out, in0, s1, s2, op0, op1), reads=R, writes=W)

    def stt(self, out, in0, scalar, in1, op0, op1, R, W):
        self.S.add("dve", lambda e: e.scalar_tensor_tensor(out, in0, scalar, in1, op0, op1), reads=R, writes=W)

    def cp(self, eng, out, in_, R, W):
        if eng == "act":
            self.S.add("act", lambda e: e.activation(out, in_, AF.Copy), reads=R, writes=W)
        else:
            self.S.add(eng, lambda e: e.tensor_copy(out, in_), reads=R, writes=W)

    def recip(self, out, in_, R, W):
        self.S.add("dve", lambda e: e.reciprocal(out, in_), reads=R, writes=W)

    def memset(self, eng, ap, val, W):
        self.S.add(eng, lambda e: e.memset(ap, val), writes=W)

    def dma(self, q, out, in_, R, W):
        self.S.add(q, lambda e: e.dma_start(out=out, in_=in_), reads=R, writes=W, dma=True)

    def setup(self):
        d = self.d
        self.dma("sp", self.cst[:], d["cst"][:, :], [], ["cst"])
        self.dma("sp", self.par[:, 0:NPAR], d["par"][:, :], [], ["par"])
        self.cp("dve", self.cstb[:], self.cst[:], ["cst"], ["cstb"])
        self.ts("dve", self.par[:, NPAR:NPAR + 48], self.par[:, 64:112], -1.0, 1.0, ALU.mult, ALU.add, ["par"], ["par2"])
        self.ts("dve", self.par[:, NPAR + 48:NPAR + 56], self.par[:, 136:144], -1.0, 1.0, ALU.mult, ALU.add, ["par"], ["par2"])
        self.ident = self.cst[:, 0:128]
        self.identb = self.cstb[:, 0:128]
        self.onesb = self.cstb[:, 640:768]
        self.blkb = self.cstb[:, 512:640]
        self.blkf = self.cst[:, 512:640]
        self.eps_rms = self.cst[:, 1344:1345]
        self.eps_gn = self.cst[:, 1345:1346]

    def load_x(self, src):
        self.wreset()
        xin = [self.walloc(D, F32) for _ in range(2)]
        for tt in range(16):
            b = xin[tt % 2]
            self.dma("sp", b, src[tt * 128:(tt + 1) * 128, :], [], [("xin", tt % 2)])
            for half in range(2):
                bk = 6 + half
                for j in range(4):
                    c = half * 4 + j
                    self.tr(self.bank(bk, j * 128, (j + 1) * 128), b[:, c * 128:(c + 1) * 128], self.ident,
                            [("xin", tt % 2), "cst"], [("ps", bk)])
                o = self.xT[:, half * 4:(half + 1) * 4, tt * 128:(tt + 1) * 128]
                i = self.bank(bk).rearrange("p (c t) -> p c t", c=4)
                self.cp("dve" if half == 0 else "act", o, i, [("ps", bk)], [("xT", tt // 4)])

    def store_x(self, dst):
        self.wreset()
        xo = [self.walloc(D, F32) for _ in range(2)]
        for tt in range(16):
            b = xo[tt % 2]
            for half in range(2):
                bk = 6 + half
                for j in range(4):
                    c = half * 4 + j
                    self.tr(self.bank(bk, j * 128, (j + 1) * 128), self.xT[:, c, tt * 128:(tt + 1) * 128], self.ident,
                            [("xT", tt // 4), "cst"], [("ps", bk)])
                self.cp("dve" if half == 0 else "act", b[:, half * 512:(half + 1) * 512], self.bank(bk),
                        [("ps", bk)], [("xo", tt % 2)])
            self.dma("sp", dst[tt * 128:(tt + 1) * 128, :], b, [("xo", tt % 2)], [])

    def prenorm(self, gvec, tmp_off):
        self.wreset(tmp_off)
        sq = self.walloc(8 * 512, BF16).rearrange("p (c t) -> p c t", c=8)
        t1 = self.walloc(512, F32)
        rstd = self.walloc(512, F32)
        for c in range(8):
            self.memset("pool", self.hT[:, c, 0:1], 0.0, [("hT", 0)])
        for tg in range(4):
            sl = slice(tg * 512, (tg + 1) * 512)
            for c in range(8):
                self.act(sq[:, c, :], self.xT[:, c, sl], AF.Square, [("xT", tg)], [("pn_sq", c)])
                self.mm(self.bank(7), self.onesb, sq[:, c, :], c == 0, c == 7, [("pn_sq", c), "cstb"], [("ps", 7)])
            self.act(t1, self.bank(7), AF.Ln, [("ps", 7), "cst"], ["pn_t1"], bias=self.eps_rms, scale=1.0 / D)
            self.act(rstd, t1, AF.Exp, ["pn_t1"], ["pn_rstd"], scale=-0.5)
            for c in range(8):
                self.stt(self.hT[:, c, 1 + tg * 512: 1 + (tg + 1) * 512], self.xT[:, c, sl], self.pcol(gvec, c), rstd,
                         ALU.mult, ALU.mult, [("xT", tg), "pn_rstd", "par"], [("hT", tg)])

    def postnorm_alloc(self, off):
        self.wreset(off)
        self.pn_o = self.walloc(8 * 512, F32).rearrange("p (c t) -> p c t", c=8)
        self.pn_sq = self.walloc(8 * 512, BF16).rearrange("p (c t) -> p c t", c=8)
        self.pn_t = self.walloc(512, F32)
        self.pn_r = self.walloc(512, F32)
        self.pn_u = self.walloc(512, F32)

    def postnorm_evac(self, bk, oc):
        self.cp("dve", self.pn_o[:, oc, :], self.bank(bk), [("ps", bk)], [("po_o", oc)])
        self.act(self.pn_sq[:, oc, :], self.pn_o[:, oc, :], AF.Square, [("po_o", oc)], [("po_sq", oc)])

    def postnorm_finish(self, gvec, tg, statbk=7):
        sl = slice(tg * 512, (tg + 1) * 512)
        for oc in range(8):
            self.mm(self.bank(statbk), self.onesb, self.pn_sq[:, oc, :], oc == 0, oc == 7, [("po_sq", oc), "cstb"], [("ps", statbk)])
        self.act(self.pn_t, self.bank(statbk), AF.Ln, [("ps", statbk), "cst"], ["po_t"], bias=self.eps_rms, scale=1.0 / D)
        self.act(self.pn_r, self.pn_t, AF.Exp, ["po_t"], ["po_r"], scale=-0.5)
        for oc in range(8):
            self.stt(self.pn_o[:, oc, :], self.pn_o[:, oc, :], self.pcol(gvec, oc), self.pn_r, ALU.mult, ALU.mult,
                     [("po_o", oc), "po_r", "par"], [("po_o", oc)])
            self.tt("pool", self.xT[:, oc, sl], self.xT[:, oc, sl], self.pn_o[:, oc, :], ALU.add,
                    [("po_o", oc), ("xT", tg)], [("xT", tg)])

    def wload(self, dst3, src2, K, key):
        self.dma("pool", dst3, src2.rearrange("(k p) f -> p k f", p=128), [], [key])

    def rwkv(self):
        d, S = self.d, self.S
        self.wreset(OFF_X)
        LIM = OFF_H
        A = lambda n, dt: self.walloc(n, dt, LIM)
        tanhT = A(2048, BF16)
        a1T = A(2048, BF16)
        sgT = A(2 * 2048, BF16).rearrange("p (c t) -> p c t", c=2)
        wd2 = A(1024, BF16)
        a2w = A(1024, BF16)
        g2w = A(2 * 1024, BF16).rearrange("p (c f) -> p c f", c=2)
        self.dma("pool", wd2[0:64, :], d["w_decay2"][:, :], [], ["wd2"])
        self.dma("pool", a2w[0:64, :], d["a2"][:, :], [], ["a2w"])
        self.dma("pool", g2w[:, 0, :], d["g2"][0:128, :], [], ["g2w"])
        self.dma("pool", g2w[0:32, 1, :], d["g2"][128:160, :], [], ["g2w"])
        wraw = [A(8 * 128, BF16).rearrange("p (k f) -> p k f", k=8) for _ in range(1)]
        wab = [[A(8 * 128, BF16).rearrange("p (k f) -> p k f", k=8) for _ in range(2)] for _ in range(3)]
        pc = A(16, F32)
        S_all = A(17 * 64, F32).rearrange("p (n i) -> p n i", n=17)
        S_bf = A(4 * 64, BF16).rearrange("p (n i) -> p n i", n=4)
        Mabr = [A(512, BF16).rearrange("p (h w t) -> p h w t", h=2, w=2) for _ in range(4)]
        Makr = [A(512, BF16).rearrange("p (h w t) -> p h w t", h=2, w=2) for _ in range(4)]
        MabT = [A(256, BF16).rearrange("p (h t) -> p h t", h=2) for _ in range(4)]
        Tm = [[A(256, BF16).rearrange("p (h t) -> p h t", h=2) for _ in range(2)] for _ in range(4)]
        W1 = [A(128, BF16).rearrange("p (h i) -> p h i", h=2) for _ in range(4)]
        GT = A(4 * 64, F32).rearrange("p (n j) -> p n j", n=4)
        Hs = A(4 * 64, F32).rearrange("p (n j) -> p n j", n=4)
        ReffT = A(512, BF16)
        YV = A(512, F32)
        self.wreset(OFF_W)
        LIM = ASZ
        PP = [[A(512, BF16).rearrange("p (h w t) -> p h w t", h=2, w=2) for _ in range(2)] for _ in range(4)]
        AU = [A(256, BF16).rearrange("p (h w i) -> p h w i", h=2, w=2) for _ in range(4)]
        tmp = [A(512, F32) for _ in range(11)]
        kksq = A(512, BF16)
        S1O = []
        for _p in range(2):
            o = dict(ar=A(4 * 256, BF16).rearrange("p (n w t) -> p n w t", n=4, w=2))
            for nm in ("bt", "kt", "bh", "kh", "vfm", "rkr", "gfm"):
                o[nm] = A(512, BF16)
            o["tok"] = A(4 * 4 * 128, BF16).rearrange("p (n w f) -> p n w f", n=4, w=4)
            S1O.append(o)
        ybuf = A(512, F32)
        gt = [A(512, F32) for _ in range(4)]

        hT = self.hT
        mu_idx = {"r": 0, "w": 1, "k": 2, "v": 3, "a": 4, "g": 5}

        def scaled(dst_a, dst_b, raw, which, ncol, key_raw, key_w):
            i = mu_idx[which]
            mu_bc = self.par[:, 64 + i * 8: 72 + i * 8].unsqueeze(2).to_broadcast([128, 8, ncol])
            om_bc = self.par[:, NPAR + i * 8: NPAR + i * 8 + 8].unsqueeze(2).to_broadcast([128, 8, ncol])
            self.tt("pool", dst_a, raw, om_bc, ALU.mult, [key_raw, "par2"], [(key_w, 0)])
            self.tt("pool", dst_b, raw, mu_bc, ALU.mult, [key_raw, "par"], [(key_w, 1)])

        def proj(out_ps, wa, wb, M, tg, keyw, bk, mlo=0):
            for k in range(8):
                self.mm(out_ps, wa[:, k, mlo:mlo + M], hT[:, k, 1 + tg * 512: 1 + (tg + 1) * 512], k == 0, False,
                        [(keyw, 0), ("hT", tg)], [("ps", bk)])
            for k in range(8):
                self.mm(out_ps, wb[:, k, mlo:mlo + M], hT[:, k, tg * 512: (tg + 1) * 512], False, k == 7,
                        [(keyw, 1), ("hT", tg), ("hT", max(tg - 1, 0)), ("hT", 0)], [("ps", bk)])

        l1a = tmp[0].bitcast(BF16)[:, 0:1024]
        lw_raw = self.bf(OFF_W, 8 * 160).rearrange("p (k f) -> p k f", k=8)
        lw_a = self.bf(OFF_W + 4096, 8 * 160).rearrange("p (k f) -> p k f", k=8)
        lw_b = self.bf(OFF_W + 8192, 8 * 160).rearrange("p (k f) -> p k f", k=8)
        for which, src, ncol in (("w", d["w_decay1"], 64), ("a", d["a1"], 64), ("g", d["g1"], 160)):
            self.wload(lw_raw[:, :, 0:ncol], src[:, :], 8, "lw_raw")
            scaled(lw_a[:, :, 0:ncol], lw_b[:, :, 0:ncol], lw_raw[:, :, 0:ncol], which, ncol, "lw_raw", "lw")
            for tg in range(4):
                sl = slice(tg * 512, (tg + 1) * 512)
                if which == "w":
                    proj(self.bank(0)[0:64, :], lw_a, lw_b, 64, tg, "lw", 0)
                    self.act(tanhT[0:64, sl], self.bank(0)[0:64, :], AF.Tanh, [("ps", 0)], ["tanhT"])
                elif which == "a":
                    proj(self.bank(1)[0:64, :], lw_a, lw_b, 64, tg, "lw", 1)
                    self.cp("dve", a1T[0:64, sl], self.bank(1)[0:64, :], [("ps", 1)], ["a1T"])
                else:
                    proj(self.bank(2), lw_a, lw_b, 128, tg, "lw", 2)
                    self.act(sgT[:, 0, sl], self.bank(2), AF.Sigmoid, [("ps", 2)], ["sgT"])
                    proj(self.bank(3)[0:32, :], lw_a, lw_b, 32, tg, "lw", 3, mlo=128)
                    self.act(sgT[0:32, 1, sl], self.bank(3)[0:32, :], AF.Sigmoid, [("ps", 3)], ["sgT"])
        S.barrier()

        su_iu = self.cstb[:, 128:384].unsqueeze(1).to_broadcast([128, 2, 256])
        slm = self.cstb[:, 384:512].unsqueeze(1).to_broadcast([128, 2, 128])
        id2 = self.cstb[:, 0:128].unsqueeze(1).to_broadcast([128, 2, 128])
        identcol = self.cst[:, 1280:1344]

        def pair(b0, lo, hi):
            assert b0 % 2 == 0
            return self.pp[b0 // 2][:, :].rearrange("p (h x) -> p h x", h=2)[:, :, lo:hi]

        hs = [slice(0, 64), slice(64, 128)]
        NS = range(4)
        CS = [slice(n * 128, (n + 1) * 128) for n in NS]
        real_add = S.add

        def record(fn):
            rec = []
            S.add = lambda *a, **k: rec.append((a, k))
            try:
                fn()
            finally:
                S.add = real_add
            return rec

        def merge(L1, L2):
            n1, n2 = len(L1), len(L2)
            i = j = 0
            while i < n1 or j < n2:
                if j >= n2 or (i < n1 and i * n2 <= j * n1):
                    a, k = L1[i]; i += 1
                else:
                    a, k = L2[j]; j += 1
                real_add(*a, **k)

        def stage1(c, tg, p):
            O = S1O[p]
            ar, bt, kt, bh, kh, vfm, rkr, gfm, tok = (O[x] for x in ("ar", "bt", "kt", "bh", "kh", "vfm", "rkr", "gfm", "tok"))
            kp = lambda nm: (nm, p)
            sl = slice(tg * 512, (tg + 1) * 512)
            if tg == 0:
                for pi, which in enumerate(("r", "k", "v")):
                    raw = wraw[0]
                    self.wload(raw, d["w_rkv"][pi, :, c * 128:(c + 1) * 128], 8, ("wraw", 0))
                    scaled(wab[pi][0], wab[pi][1], raw, which, 128, ("wraw", 0), ("wab", pi))
                self.memset("pool", S_all[:, 0, :], 0.0, [("S_all", 0)])
            t = tmp
            K = lambda i: ("t", i)
            BW, BA, BK, BS, BR, BV, BG = 5, 6, 7, 5, 6, 7, 5
            self.mm(self.bank(BW), wd2[0:64, c * 128:(c + 1) * 128], tanhT[0:64, sl], True, True, ["wd2", "tanhT"], [("ps", BW)])
            self.act(t[0], self.bank(BW), AF.Sigmoid, [("ps", BW), "par"], [K(0)], bias=self.pcol(14, c))
            self.mm(self.bank(BA), a2w[0:64, c * 128:(c + 1) * 128], a1T[0:64, sl], True, True, ["a2w", "a1T"], [("ps", BA)])
            self.act(t[5], self.bank(BA), AF.Sigmoid, [("ps", BA), "par"], [K(5)], bias=self.pcol(15, c))
            proj(self.bank(BK), wab[1][0], wab[1][1], 128, tg, ("wab", 1), BK)
            k_ps = self.bank(BK)
            for n in range(4):
                cs = CS[n]
                S.add("dve", lambda e, o=t[1][:, cs], i0=t[0][:, cs], z=self.cst[:, 1346:1347].to_broadcast([128, 128]):
                      e.tensor_tensor_scan(o, i0, z, 0.0, ALU.add, ALU.add), reads=[K(0), "cst"], writes=[K(1)])
            self.tt("pool", t[0], t[1], t[0], ALU.subtract, [K(0), K(1)], [K(0)])
            self.act(t[2], t[1], AF.Exp, [K(1)], [K(2)], scale=C0)
            self.act(t[3], t[1], AF.Exp, [K(1)], [K(3)], scale=-C0)
            self.act(t[0], t[0], AF.Exp, [K(0)], [K(0)], scale=-C0)
            self.cp("pool", pc[:, tg * 4:(tg + 1) * 4], t[3].rearrange("p (n t) -> p n t", n=4)[:, :, 127], [K(3)], [("pc", tg)])
            self.tt("dve", t[4].rearrange("p (n t) -> p n t", n=4), t[2].rearrange("p (n t) -> p n t", n=4),
                    pc[:, tg * 4:(tg + 1) * 4].unsqueeze(2).to_broadcast([128, 4, 128]), ALU.mult, [K(2), ("pc", tg)], [K(4)])
            self.act(t[6], k_ps, AF.Identity, [("ps", BK), "par"], [K(6)], scale=self.pcol(16, c))
            self.ts("dve", t[8], t[5], self.pcol(17, c), self.par[:, NPAR + 48 + c:NPAR + 49 + c], ALU.mult, ALU.add, [K(5), "par", "par2"], [K(8)])
            self.tt("dve", t[8], k_ps, t[8], ALU.mult, [("ps", BK), K(8)], [K(8)])
            self.act(kksq, t[6], AF.Square, [K(6)], ["kksq"])
            self.mm(self.bank(BS), self.blkb, kksq, True, True, ["kksq", "cstb"], [("ps", BS)])
            proj(self.bank(BR), wab[0][0], wab[0][1], 128, tg, ("wab", 0), BR)
            proj(self.bank(BV), wab[2][0], wab[2][1], 128, tg, ("wab", 2), BV)
            r_ps, v_ps = self.bank(BR), self.bank(BV)
            self.ts("dve", t[7], self.bank(BS), 1e-19, None, ALU.max, None, [("ps", BS)], [K(7)])
            self.act(t[7], t[7], AF.Ln, [K(7)], [K(7)])
            self.act(t[7], t[7], AF.Exp, [K(7)], [K(7)], scale=-0.5)
            self.tt("pool", t[6], t[6], t[7], ALU.mult, [K(6), K(7)], [K(6)])
            self.mm(self.bank(BG), g2w[:, 0, c * 128:(c + 1) * 128], sgT[:, 0, sl], True, False, ["g2w", "sgT"], [("ps", BG)])
            self.mm(self.bank(BG), g2w[0:32, 1, c * 128:(c + 1) * 128], sgT[0:32, 1, sl], False, True, ["g2w", "sgT"], [("ps", BG)])
            self.stt(ar[:, :, 0, :], t[6].rearrange("p (n t) -> p n t", n=4), -1.0, t[0].rearrange("p (n t) -> p n t", n=4),
                     ALU.mult, ALU.mult, [K(6), K(0)], [kp("ar_a")])
            self.tt("pool", t[6], t[6], t[5], ALU.mult, [K(6), K(5)], [K(6)])
            self.tt("dve", bt, t[6], t[2], ALU.mult, [K(6), K(2)], [kp("bt")])
            self.tt("pool", bh, t[6], t[4], ALU.mult, [K(6), K(4)], [kp("bh")])
            self.tt("pool", kt, t[8], t[2], ALU.mult, [K(8), K(2)], [kp("kt")])
            self.tt("pool", kh, t[8], t[4], ALU.mult, [K(8), K(4)], [kp("kh")])
            self.tt("dve", ar[:, :, 1, :], r_ps.rearrange("p (n t) -> p n t", n=4), t[3].rearrange("p (n t) -> p n t", n=4),
                    ALU.mult, [("ps", BR), K(3)], [kp("ar_r")])
            self.stt(rkr, r_ps, self.pcol(18, c), t[8], ALU.mult, ALU.mult, [("ps", BR), K(8), "par"], [kp("rkr")])
            self.cp("act", vfm, v_ps, [("ps", BV)], [kp("vfm")])
            self.cp("act", gfm, self.bank(BG), [("ps", BG)], [kp("gfm")])
            for n in range(4):
                cs = CS[n]
                tb = (6, 7, 5, 6)[n]
                srcs = [(vfm[:, cs], kp("vfm")), (ar[:, n, 0, :], kp("ar_a")), (bh[:, cs], kp("bh")), (kh[:, cs], kp("kh"))]
                for w, (src, key) in enumerate(srcs):
                    self.tr(self.bankb(tb, w * 128, (w + 1) * 128), src, self.identb, [key, "cstb"], [("ps", tb)])
                self.cp("act", tok[:, n, :, :], self.bankb(tb, 0, 512).rearrange("p (w f) -> p w f", w=4), [("ps", tb)], [("tok", p, n)])

        def stage2(c, tg, p):
            O = S1O[p]
            ar, bt, kt, bh, kh, vfm, rkr, gfm, tok = (O[x] for x in ("ar", "bt", "kt", "bh", "kh", "vfm", "rkr", "gfm", "tok"))
            kp = lambda nm: (nm, p)
            sl = slice(tg * 512, (tg + 1) * 512)
            for rnd in range(2):
                for n in (2 * rnd, 2 * rnd + 1):
                    o = (n % 2) * 256
                    for h in range(2):
                        rhs = ar[hs[h], n, :, :]
                        self.mm(self.bank(h, o, o + 256), bt[hs[h], CS[n]], rhs, True, True, [kp("bt"), kp("ar_a"), kp("ar_r")], [("ps", h)])
                        self.mm(self.bank(2 + h, o, o + 256), kt[hs[h], CS[n]], rhs, True, True, [kp("kt"), kp("ar_a"), kp("ar_r")], [("ps", 2 + h)])
                for n in (2 * rnd, 2 * rnd + 1):
                    o = (n % 2) * 256
                    self.tt("dve", Mabr[n].rearrange("p h w t -> p h (w t)"), pair(0, o, o + 256), su_iu, ALU.mult,
                            [("ps", 0), ("ps", 1), "cstb"], [("Mabr", n)])
                    self.tt("dve", Makr[n].rearrange("p h w t -> p h (w t)"), pair(2, o, o + 256), su_iu, ALU.mult,
                            [("ps", 2), ("ps", 3), "cstb"], [("Makr", n)])
            for n in NS:
                for h in range(2):
                    self.mm(self.bank(h, n * 128, (n + 1) * 128), ar[hs[h], n, 0, :], bt[hs[h], CS[n]], True, True,
                            [kp("bt"), kp("ar_a")], [("ps", h)])
            for n in NS:
                self.tt("dve", MabT[n], pair(0, n * 128, (n + 1) * 128), slm, ALU.mult, [("ps", 0), ("ps", 1), "cstb"], [("MabT", n)])
            cur = 0
            for n in NS:
                self.tt("dve", Tm[n][cur], Mabr[n][:, :, 0, :], id2, ALU.add, [("Mabr", n), "cstb"], [("Tm", n, cur)])
            Pk = [lambda h, n=n: Mabr[n][:, h, 0, :] for n in NS]
            PkT = [lambda h, n=n: MabT[n][:, h, :] for n in NS]
            Rk = [[("Mabr", n), ("MabT", n)] for n in NS]
            def squares(s_, Pk, PkT, Rk):
                for n in NS:
                    for h in range(2):
                        o = h * 256
                        if s_ < 6:
                            self.mm(self.bank(n, o, o + 128), PkT[n](h), Pk[n](h), True, True, Rk[n], [("ps", n)])
                        self.mm(self.bank(n, o + 128, o + 256), Pk[n](h), PkT[n](h), True, True, Rk[n], [("ps", n)])

            def evacpp(s_):
                pq = s_ % 2
                for n in NS:
                    self.cp("act", PP[n][pq].rearrange("p h w t -> p (h w t)"), self.bank(n), [("ps", n)], [("PP", n, pq)])
                return ([lambda h, n=n, pq=pq: PP[n][pq][:, h, 0, :] for n in NS],
                        [lambda h, n=n, pq=pq: PP[n][pq][:, h, 1, :] for n in NS],
                        [[("PP", n, pq)] for n in NS])

            squares(1, Pk, PkT, Rk)
            Pk, PkT, Rk = evacpp(1)
            for s_ in range(1, 7):
                pq = s_ % 2
                if s_ < 6:
                    squares(s_ + 1, Pk, PkT, Rk)
                for rnd in range(2):
                    for n in (2 * rnd, 2 * rnd + 1):
                        for h in range(2):
                            o = ((n % 2) * 2 + h) * 128
                            self.mm(self.bank(4, o, o + 128), PkT[n](h), Tm[n][cur][:, h, :], True, True,
                                    [("PP", n, pq), ("Tm", n, cur)], [("ps", 4)])
                    for n in (2 * rnd, 2 * rnd + 1):
                        o = (n % 2) * 256
                        self.tt("dve", Tm[n][1 - cur].rearrange("p h t -> p (h t)"), self.bank(4, o, o + 256),
                                Tm[n][cur].rearrange("p h t -> p (h t)"), ALU.add, [("ps", 4), ("Tm", n, cur)], [("Tm", n, 1 - cur)])
                cur = 1 - cur
                if s_ < 6:
                    Pk, PkT, Rk = evacpp(s_ + 1)
            for n in NS:
                for h in range(2):
                    o = (n * 2 + h) * 64
                    self.mm(self.bank(0, o, o + 64), Makr[n][:, h, 0, :], tok[:, n, 0, hs[h]], True, True, [("Makr", n), ("tok", p, n)], [("ps", 0)])
            for n in NS:
                self.cp("act", W1[n].rearrange("p h i -> p (h i)"), self.bank(0, n * 128, (n + 1) * 128), [("ps", 0)], [("W1", n)])
            for n in NS:
                TmF, kT_ = Tm[n][cur], ("Tm", n, cur)
                bb = 1 + n // 2
                for h in range(2):
                    o = ((n % 2) * 2 + h) * 128
                    self.mm(self.bank(bb, o, o + 64), TmF[:, h, :], tok[:, n, 1, hs[h]], True, True, [kT_, ("tok", p, n)], [("ps", bb)])
                    self.mm(self.bank(bb, o + 64, o + 128), TmF[:, h, :], W1[n][:, h, :], True, True, [kT_, ("W1", n)], [("ps", bb)])
            for n in NS:
                bb = 1 + n // 2
                o = (n % 2) * 256
                self.cp("act", AU[n].rearrange("p h w i -> p (h w i)"), self.bank(bb, o, o + 256), [("ps", bb)], [("AU", n)])
            for n in NS:
                for h in range(2):
                    Ae, UV = AU[n][:, h, 0, :], AU[n][:, h, 1, :]
                    ka = ("AU", n)
                    tk = ("tok", p, n)
                    self.mm(self.bank(3, n * 128, (n + 1) * 128)[hs[h], :], Ae, Mabr[n][:, h, 1, :], True, True, [ka, ("Mabr", n)], [("ps", 3)])
                    self.mm(self.bank(4, n * 64, (n + 1) * 64)[hs[h], :], Ae, tok[:, n, 2, hs[h]], True, True, [ka, tk], [("ps", 4)])
                    self.mm(self.bank(4, 256 + n * 64, 256 + (n + 1) * 64)[hs[h], :], tok[:, n, 2, hs[h]], UV, True, False, [ka, tk], [("ps", 4)])
                    self.mm(self.bank(4, 256 + n * 64, 256 + (n + 1) * 64)[hs[h], :], tok[:, n, 3, hs[h]], tok[:, n, 0, hs[h]], False, True, [tk], [("ps", 4)])
                    self.mm(self.bank(0, n * 128, (n + 1) * 128)[hs[h], :], UV, Mabr[n][:, h, 1, :], True, False, [ka, ("Mabr", n)], [("ps", 0)])
                    self.mm(self.bank(0, n * 128, (n + 1) * 128)[hs[h], :], tok[:, n, 0, hs[h]], Makr[n][:, h, 1, :], False, True, [tk, ("Makr", n)], [("ps", 0)])
            self.tt("dve", ReffT, self.bank(3), ar[:, :, 1, :], ALU.add, [("ps", 3), kp("ar_r")], [("ReffT", n) for n in NS])
            for n in NS:
                gn = tg * 4 + n
                self.stt(GT[:, n, :], identcol, pc[:, gn:gn + 1], self.bank(4, n * 64, (n + 1) * 64), ALU.mult, ALU.add,
                         [("ps", 4), ("pc", tg), "cst"], [("GT", n)])
            self.cp("act", Hs.rearrange("p n j -> p (n j)"), self.bank(4, 256, 512), [("ps", 4)], [("Hs", n) for n in NS])
            self.cp("act", YV, self.bank(0), [("ps", 0)], [("YV", n) for n in NS])
            for n in NS:
                gn = tg * 4 + n
                for h in range(2):
                    ob_ = self.bank(1 + h, n * 64, (n + 1) * 64)[hs[h], :]
                    self.mm(ob_, GT[hs[h], n, :], S_all[hs[h], gn, :], True, True, [("GT", n), ("S_all", gn)], [("ps", 1 + h)])
                    self.tt("dve", S_all[hs[h], gn + 1, :], ob_, Hs[hs[h], n, :], ALU.add,
                            [("ps", 1 + h), ("Hs", n)], [("S_all", gn + 1)])
                self.cp("act", S_bf[:, n, :], S_all[:, gn, :], [("S_all", gn)], [("S_bf", n)])
            for n in NS:
                for h in range(2):
                    self.mm(self.bank(3 + h)[hs[h], CS[n]], S_bf[hs[h], n, :], ReffT[hs[h], CS[n]], True, True,
                            [("S_bf", n), ("ReffT", n)], [("ps", 3 + h)])
            for h in range(2):
                self.tt("dve", ybuf[hs[h], :], self.bank(3 + h)[hs[h], :], YV[hs[h], :], ALU.add,
                        [("ps", 3 + h)] + [("YV", n) for n in NS], [("y", h)])
            Ky = [("y", 0), ("y", 1)]
            self.tt("dve", gt[0], ybuf, ybuf, ALU.mult, Ky, ["g0"])
            self.mm(self.bank(0), self.blkf, ybuf, True, True, Ky + ["cst"], [("ps", 0)])
            self.mm(self.bank(1), self.blkf, gt[0], True, True, ["g0", "cst"], [("ps", 1)])
            self.mm(self.bank(2), self.blkb, rkr, True, True, [kp("rkr"), "cstb"], [("ps", 2)])
            self.act(gt[1], self.bank(0), AF.Copy, [("ps", 0)], ["g1"], scale=1.0 / 64)
            self.tt("dve", gt[2], gt[1], gt[1], ALU.mult, ["g1"], ["g2"])
            self.stt(gt[2], self.bank(1), 1.0 / 64, gt[2], ALU.mult, ALU.subtract, [("ps", 1), "g2"], ["g2"])
            self.act(gt[2], gt[2], AF.Ln, ["g2", "cst"], ["g2"], bias=self.eps_gn)
            self.act(gt[2], gt[2], AF.Exp, ["g2"], ["g2"], scale=-0.5)
            self.tt("dve", gt[0], ybuf, gt[1], ALU.subtract, Ky + ["g1"], ["g0"])
            self.tt("dve", gt[0], gt[0], gt[2], ALU.mult, ["g0", "g2"], ["g0"])
            self.ts("dve", gt[0], gt[0], self.pcol(19, c), self.pcol(20, c), ALU.mult, ALU.add, ["g0", "par"], ["g0"])
            self.tt("dve", gt[3], self.bank(2), vfm, ALU.mult, [("ps", 2), kp("vfm")], ["g3"])
            self.tt("dve", gt[0], gt[0], gt[3], ALU.add, ["g0", "g3"], ["g0"])
            self.tt("dve", self.zT[:, c, sl], gt[0], gfm, ALU.mult, ["g0", kp("gfm")], [("zT", tg)])

        seq = [(c, tg) for c in range(8) for tg in range(4)]
        stage1(*seq[0], 0)
        for k in range(len(seq)):
            L1 = record(lambda: stage1(*seq[k + 1], (k + 1) % 2)) if k + 1 < len(seq) else []
            L2 = record(lambda: stage2(*seq[k], k % 2))
            merge(L1, L2)

    def rwkv_out(self, x_src):
        d = self.d
        self.S.barrier()
        self.load_x(x_src)
        self.wreset(OFF_W + 8192)
        wo = self.walloc(8 * 1024, BF16).rearrange("p (k f) -> p k f", k=8)
        self.wload(wo, d["w_o_rwkv"][:, :], 8, "wo")
        self.postnorm_alloc(self.wtop)
        for tg in range(4):
            sl = slice(tg * 512, (tg + 1) * 512)
            for oc in range(8):
                bk = oc % 4
                for k in range(8):
                    self.mm(self.bank(bk), wo[:, k, oc * 128:(oc + 1) * 128], self.zT[:, k, sl], k == 0, k == 7,
                            ["wo", ("zT", tg)], [("ps", bk)])
                self.postnorm_evac(bk, oc)
            self.postnorm_finish(1, tg)

    def ffn(self, l):
        d, S = self.d, self.S
        S.barrier()
        self.prenorm(4 * l + 2, OFF_W)
        S.barrier()
        G = self.bf(OFF_Z, 22 * 1024).rearrange("p (k t) -> p k t", k=22)
        FW = OFF_Z + 22 * 1024 * 2
        self.wreset(FW)
        halo = self.walloc(44 * 2, F32).rearrange("p (c t) -> p c t", c=44)
        wdn = [self.walloc(22 * 128, BF16).rearrange("p (k f) -> p k f", k=22) for _ in range(2)]
        TOP = self.wtop
        cw = lambda k, ch: self.par[:, 176 + (l * 3 + k) * 44 + ch: 176 + (l * 3 + k) * 44 + ch + 1]
        cb = lambda ch: self.par[:, 440 + l * 44 + ch: 440 + l * 44 + ch + 1]
        hT = self.hT
        NW = 8
        PF = 5
        for hf in range(2):
            self.wreset(TOP)
            wup = [self.walloc(8 * 128, BF16).rearrange("p (k f) -> p k f", k=8) for _ in range(NW)]
            cv = [self.walloc(1024, F32) for _ in range(4)]
            ga = [self.walloc(1024, F32) for _ in range(2)]
            units = [(i, w) for i in range(22) for w in range(2)]

            def issue_w(n):
                i, w = units[n]
                ch = w * 22 + i
                self.wload(wup[n % NW], d["w_up"][l, :, ch * 128:(ch + 1) * 128], 8, ("wup", n % NW))

            for n in range(min(PF, len(units))):
                issue_w(n)
            for n, (i, w) in enumerate(units):
                if n + PF < len(units):
                    issue_w(n + PF)
                ch = w * 22 + i
                wt, kw = wup[n % NW], ("wup", n % NW)
                b0 = (n % 4) * 2
                for g in range(2):
                    tg = hf * 2 + g
                    for k in range(8):
                        self.mm(self.bank(b0 + g), wt[:, k, :], hT[:, k, 1 + tg * 512: 1 + (tg + 1) * 512], k == 0, k == 7,
                                [kw, ("hT", tg)], [("ps", b0 + g)])
                ps = self.pp[b0 // 2][:, 0:1024]
                PK = [("ps", b0), ("ps", b0 + 1)]
                Cv, kc = cv[n % 4], ("cv", n % 4)
                self.act(Cv, ps, AF.Identity, PK + ["par"], [kc], bias=cb(ch), scale=cw(2, ch))
                self.stt(Cv[:, 1:1024], ps[:, 0:1023], cw(1, ch), Cv[:, 1:1024], ALU.mult, ALU.add, PK + [kc, "par"], [kc])
                self.stt(Cv[:, 2:1024], ps[:, 0:1022], cw(0, ch), Cv[:, 2:1024], ALU.mult, ALU.add, PK + [kc, "par"], [kc])
                if hf == 0:
                    self.cp("dve", halo[:, ch, :], ps[:, 1022:1024], PK, [("halo", ch)])
                else:
                    self.stt(Cv[:, 0:2], halo[:, ch, :], cw(0, ch), Cv[:, 0:2], ALU.mult, ALU.add, [("halo", ch), kc, "par"], [kc])
                    self.stt(Cv[:, 0:1], halo[:, ch, 1:2], cw(1, ch), Cv[:, 0:1], ALU.mult, ALU.add, [("halo", ch), kc, "par"], [kc])
                if w == 1:
                    ca, cu = cv[(n - 1) % 4], cv[n % 4]
                    gt_, kg = ga[i % 2], ("ga", i % 2)
                    self.act(gt_, ca, AF.Gelu_apprx_tanh, [("cv", (n - 1) % 4)], [kg])
                    self.tt("pool", G[:, i, :], gt_, cu, ALU.mult, [kg, ("cv", n % 4)], [("G", i)])
            self.wload(wdn[0], d["w_down"][l, :, 0:128], 22, ("wdn", 0))
            S.barrier()
            self.wreset(TOP)
            o_sb = self.walloc(8 * 1024, F32).rearrange("p (c t) -> p c t", c=8)
            osq = [self.walloc(512, BF16) for _ in range(4)]
            pn_t = self.walloc(1024, F32)
            pn_r = self.walloc(1024, F32)
            pend = []

            def flush():
                for (oc_, g_, q_) in pend:
                    self.mm(self.bank(6 + g_), self.onesb, osq[q_], oc_ == 0, oc_ == 7, [("osq", q_), "cstb"], [("ps", 6 + g_)])
                pend.clear()

            for oc in range(8):
                if oc + 1 < 8:
                    self.wload(wdn[(oc + 1) % 2], d["w_down"][l, :, (oc + 1) * 128:(oc + 2) * 128], 22, ("wdn", (oc + 1) % 2))
                wd, kw = wdn[oc % 2], ("wdn", oc % 2)
                for g in range(2):
                    bk = (oc % 2) * 2 + g
                    for k in range(22):
                        self.mm(self.bank(bk), wd[:, k, :], G[:, k, g * 512:(g + 1) * 512], k == 0, k == 21,
                                [kw, ("G", k)], [("ps", bk)])
                flush()
                for g in range(2):
                    bk = (oc % 2) * 2 + g
                    q = (oc * 2 + g) % 4
                    self.cp("dve", o_sb[:, oc, g * 512:(g + 1) * 512], self.bank(bk), [("ps", bk)], [("o_sb", oc, g)])
                    self.act(osq[q], o_sb[:, oc, g * 512:(g + 1) * 512], AF.Square, [("o_sb", oc, g)], [("osq", q)])
                    pend.append((oc, g, q))
            flush()
            gvec = 4 * l + 3
            for g in range(2):
                tg = hf * 2 + g
                sl = slice(tg * 512, (tg + 1) * 512)
                gs = slice(g * 512, (g + 1) * 512)
                self.act(pn_t[:, gs], self.bank(6 + g), AF.Ln, [("ps", 6 + g), "cst"], [("pn_t", g)], bias=self.eps_rms, scale=1.0 / D)
                self.act(pn_r[:, gs], pn_t[:, gs], AF.Exp, [("pn_t", g)], [("pn_r", g)], scale=-0.5)
                for oc in range(8):
                    self.stt(o_sb[:, oc, gs], o_sb[:, oc, gs], self.pcol(gvec, oc), pn_r[:, gs], ALU.mult, ALU.mult,
                             [("o_sb", oc, g), ("pn_r", g), "par"], [("o_sb", oc, g)])
                    self.tt("pool", self.xT[:, oc, sl], self.xT[:, oc, sl], o_sb[:, oc, gs], ALU.add,
                            [("o_sb", oc, g), ("xT", tg)], [("xT", tg)])
            S.barrier()

    def kv(self):
        d, S = self.d, self.S
        S.barrier()
        self.prenorm(21, OFF_W)
        self.kT = self.bf(OFF_Z, 4 * T).rearrange("p (h t) -> p h t", h=4)
        self.vtok = self.bf(OFF_Z + 16384, 16 * 256).rearrange("p (b f) -> p b f", b=16)
        self.wreset(OFF_W + 16384)
        wkv = self.walloc(8 * 512, BF16).rearrange("p (k f) -> p k f", k=8)
        wkd = self.walloc(8 * 512, BF16).rearrange("p (k h e) -> p k h e", k=8, h=4)
        self.wload(wkv, d["w_kv"][:, :], 8, "wkv")
        kview = wkv[:, :, 0:256].rearrange("p k (h e) -> p k h e", h=4)
        self.cp("pool", wkd[:, :, :, 0:64], kview, ["wkv"], ["wkd"])
        self.cp("pool", wkd[:, :, :, 64:128], kview, ["wkv"], ["wkd"])
        hT = self.hT
        for kvh in range(4):
            for tg in range(4):
                bk = (kvh * 4 + tg) % 4
                for k in range(8):
                    self.mm(self.bank(bk), wkd[:, k, kvh, :], hT[:, k, 1 + tg * 512: 1 + (tg + 1) * 512], k == 0, k == 7,
                            ["wkd", ("hT", tg)], [("ps", bk)])
                self.cp("act" if tg % 2 else "dve", self.kT[:, kvh, tg * 512:(tg + 1) * 512], self.bank(bk), [("ps", bk)], ["kT"])
        for tb in range(16):
            bk = 4 + tb % 4
            for k in range(8):
                self.mm(self.bank(bk, 0, 256), hT[:, k, 1 + tb * 128: 1 + (tb + 1) * 128], wkv[:, k, 256:512], k == 0, k == 7,
                        ["wkv", ("hT", tb // 4)], [("ps", bk)])
            self.cp("act" if tb % 2 else "dve", self.vtok[:, tb, :], self.bank(bk, 0, 256), [("ps", bk)], ["vtok"])

    def attn(self):
        d, S = self.d, self.S
        S.barrier()
        self.prenorm(4, OFF_W)
        S.barrier()
        self.wreset(OFF_W)
        qT = self.walloc(8 * T, BF16).rearrange("p (c t) -> p c t", c=8)
        wo = self.walloc(8 * 1024, BF16).rearrange("p (k f) -> p k f", k=8)
        TOP = self.wtop
        wq = [self.walloc(8 * 128, BF16).rearrange("p (k f) -> p k f", k=8) for _ in range(2)]
        hT = self.hT
        self.wload(wo, d["w_o_attn"][:, :], 8, "wo")
        for oc in range(8):
            w = wq[oc % 2]
            self.wload(w, d["w_q"][:, oc * 128:(oc + 1) * 128], 8, ("wq", oc % 2))
            for tg in range(4):
                bk = (oc * 4 + tg) % 8
                for k in range(8):
                    self.mm(self.bank(bk), w[:, k, :], hT[:, k, 1 + tg * 512: 1 + (tg + 1) * 512], k == 0, k == 7,
                            [("wq", oc % 2), ("hT", tg)], [("ps", bk)])
                self.cp("act" if tg % 2 else "dve", qT[:, oc, tg * 512:(tg + 1) * 512], self.bank(bk), [("ps", bk)], [("qT", oc)])
        S.barrier()
        self.wreset(TOP)
        ND = 4
        ssb = [self.walloc(2 * 260, F32).rearrange("p (h k) -> p h k", h=2) for _ in range(ND)]
        Pb = [self.walloc(2 * 260, BF16).rearrange("p (h k) -> p h k", h=2) for _ in range(ND)]
        PT = [self.walloc(512, BF16).rearrange("p (h j q) -> p h j q", h=2, j=2) for _ in range(ND)]
        sm = [self.walloc(8, F32) for _ in range(ND)]
        otok = [self.walloc(1024, BF16) for _ in range(2)]
        oT = self.bf(OFF_Z + 24576, 8 * 512).rearrange("p (c t) -> p c t", c=8)
        self.postnorm_alloc(OFF_H)
        amask = self.cst[:, 768:1024].unsqueeze(1).to_broadcast([128, 2, 256])
        amask0 = self.cst[:, 1024:1280].unsqueeze(1).to_broadcast([128, 2, 256])
        sinks = self.par[:, 528:544]
        units = [(b, c) for b in range(16) for c in range(8)]
        NU = len(units)

        def ctx(i):
            b, c = units[i]
            u = i % ND
            return dict(b=b, c=c, kvh=c // 2, u=u, v2=i % 2, kb0=max(b - 1, 0), sb=ssb[u], pb=Pb[u], pt=PT[u], sm=sm[u], ob=otok[b % 2])

        def ppair(b0, lo, hi):
            return self.pp[b0 // 2][:, :].rearrange("p (h x) -> p h x", h=2)[:, :, lo:hi]

        def S0(i):
            x = ctx(i)
            b, c, u = x["b"], x["c"], x["u"]
            b0 = 2 * x["v2"]
            for hh in range(2):
                hsl = slice(hh * 64, hh * 64 + 64)
                self.mm(self.bank(b0 + hh, 0, 256), qT[hsl, c, b * 128:(b + 1) * 128], self.kT[hsl, x["kvh"], x["kb0"] * 128:(x["kb0"] + 2) * 128],
                        True, True, [("qT", c), "kT"], [("ps", b0 + hh)])
            self.cp("pool", x["sb"][:, :, 256], sinks[:, 2 * c:2 * c + 2], ["par"], [("ssbk", u)])
            self.stt(x["sb"][:, :, 0:256], ppair(b0, 0, 256), 0.125, amask0 if b == 0 else amask, ALU.mult, ALU.add,
                     [("ps", b0), ("ps", b0 + 1), "cst"], [("ssb", u)])
            self.S.add("dve", lambda e, o=x["sm"][:, 0:2], i_=x["sb"][:, :, 0:257]: e.tensor_reduce(o, i_, AX.X, ALU.max, negate=True),
                       reads=[("ssb", u), ("ssbk", u)], writes=[("sm", u)])

        def S1(i):
            x = ctx(i)
            u = x["u"]
            for hh in range(2):
                self.act(x["pb"][:, hh, 0:257], x["sb"][:, hh, 0:257], AF.Exp, [("ssb", u), ("ssbk", u), ("sm", u)], [("Pb", u), ("sm2", u)],
                         bias=x["sm"][:, hh:hh + 1], accum_out=x["sm"][:, 2 + hh:3 + hh])
            self.recip(x["sm"][:, 4:6], x["sm"][:, 2:4], [("sm2", u)], [("sm3", u)])

        def S1b(i):
            x = ctx(i)
            u = x["u"]
            bt_ = 4 + x["v2"]
            for hh in range(2):
                for j in range(2):
                    o = (hh * 2 + j) * 128
                    self.tr(self.bankb(bt_, o, o + 128), x["pb"][:, hh, j * 128:(j + 1) * 128], self.identb,
                            [("Pb", u), "cstb"], [("ps", bt_)])
            self.cp("act", x["pt"].rearrange("p h j q -> p (h j q)"), self.bankb(bt_, 0, 512), [("ps", bt_)], [("PT", u)])

        def S2(i):
            x = ctx(i)
            u, b, c, kvh = x["u"], x["b"], x["c"], x["kvh"]
            bo = 6 + x["v2"]
            for hh in range(2):
                for j in range(2):
                    self.mm(self.bank(bo, hh * 64, hh * 64 + 64), x["pt"][:, hh, j, :], self.vtok[:, x["kb0"] + j, kvh * 64:(kvh + 1) * 64],
                            j == 0, j == 1, [("PT", u), "vtok"], [("ps", bo)])
            self.tt("dve", x["ob"][:, c * 128:(c + 1) * 128].rearrange("p (h e) -> p h e", h=2),
                    self.bank(bo, 0, 128).rearrange("p (h e) -> p h e", h=2),
                    x["sm"][:, 4:6].unsqueeze(2).to_broadcast([128, 2, 64]), ALU.mult,
                    [("ps", bo), ("sm3", u)], [("otok", b % 2)])
            if c == 7:
                tail(b)

        def tail(b):
            ob = otok[b % 2]
            for half in range(2):
                bk = 4 + half
                for j in range(4):
                    cc = half * 4 + j
                    self.tr(self.bankb(bk, j * 128, (j + 1) * 128), ob[:, cc * 128:(cc + 1) * 128], self.identb,
                            [("otok", b % 2), "cstb"], [("ps", bk)])
                self.cp("act", oT[:, half * 4:(half + 1) * 4, (b % 4) * 128:(b % 4 + 1) * 128],
                        self.bankb(bk, 0, 512).rearrange("p (c t) -> p c t", c=4), [("ps", bk)], ["oT"])
            if b % 4 == 3:
                tg = b // 4
                for oc in range(8):
                    bk = oc % 4
                    for k in range(8):
                        self.mm(self.bank(bk), wo[:, k, oc * 128:(oc + 1) * 128], oT[:, k, :], k == 0, k == 7, ["wo", "oT"], [("ps", bk)])
                    self.postnorm_evac(bk, oc)
                self.postnorm_finish(5, tg, statbk=7)

        for step in range(NU + 3):
            if step < NU:
                S0(step)
            if 0 <= step - 3 < NU:
                S2(step - 3)
            if 0 <= step - 1 < NU:
                S1(step - 1)
            if 0 <= step - 2 < NU:
                S1b(step - 2)


def build(stages=("rwkv", "ffn0", "kv", "attn", "ffn1"), dbg=()):
    nc = bass.Bass("TRN2", target_bir_lowering=False)
    dram = {}

    def din(name, shape):
        dram[name] = nc.dram_tensor(name, list(shape), F32, kind="ExternalInput").ap()

    din("x", [T, D]); din("cst", [128, NCST]); din("par", [128, NPAR])
    din("w_rkv", [3, D, D]); din("w_decay1", [D, 64]); din("w_decay2", [64, D]); din("a1", [D, 64]); din("a2", [64, D])
    din("g1", [D, 160]); din("g2", [160, D]); din("w_o_rwkv", [D, D]); din("w_kv", [D, 512]); din("w_q", [D, D])
    din("w_o_attn", [D, D]); din("w_up", [2, D, 2 * DFF]); din("w_down", [2, DFF, D])
    y = nc.dram_tensor("y", [T, D], F32, kind="ExternalOutput").ap()
    with ExitStack() as es:
        kb = KB(nc, es, dram)
        kb.setup()
        kb.load_x(dram["x"])
        if "rwkv" in stages:
            kb.S.barrier()
            kb.prenorm(0, OFF_W)
            kb.S.barrier()
            kb.rwkv()
            kb.rwkv_out(dram["x"])
        if "t_wload" in stages:
            kb.S.barrier()
            kb.wreset(OFF_W)
            wt = kb.walloc(8 * 128, BF16).rearrange("p (k f) -> p k f", k=8)
            kb.wload(wt, dram["w_q"][:, 0:128], 8, "twl")
        if "t_prenorm" in stages:
            kb.S.barrier()
            kb.prenorm(0, OFF_W)
        if "t_pool" in stages:
            kb.S.barrier()
            kb.wreset(OFF_W)
            tt_ = kb.walloc(512, F32)
            kb.memset("pool", tt_, 1.0, ["tp"])
            kb.tt("pool", tt_, tt_, tt_, ALU.mult, ["tp"], ["tp"])
        if "ffn0" in stages:
            kb.ffn(0)
        if "kv" in stages:
            kb.kv()
        if "attn" in stages:
            kb.attn()
        if "ffn1" in stages:
            kb.ffn(1)
        kb.S.barrier()
        kb.store_x(y)
        kb.S.emit(nc, es)
    return nc


def make_consts():
    c = np.zeros((128, NCST), np.float32)
    i = np.arange(128)
    c[:, 0:128] = np.eye(128)
    c[:, 128:256] = (i[:, None] < i[None, :])
    c[:, 256:384] = (i[:, None] <= i[None, :])
    c[:, 384:512] = (i[:, None] > i[None, :])
    c[:, 512:640] = (i[:, None] // 64 == i[None, :] // 64)
    c[:, 640:768] = 1.0
    q = i[:, None]
    kk = np.arange(256)[None, :]
    valid = (kk > q) & (kk <= q + 128)
    c[:, 768:1024] = np.where(valid, 0.0, -1e30)
    valid0 = (kk <= q)
    c[:, 1024:1280] = np.where(valid0, 0.0, -1e30)
    c[:, 1280:1344] = (i[:, None] % 64 == np.arange(64)[None, :])
    c[:, 1344] = 1e-6
    c[:, 1345] = 64e-5
    c[:, 1346] = 0.0
    return c


def make_par(inp):
    p = np.zeros((128, NPAR), np.float32)

    def put(v, vec):
        p[:, v * 8:(v + 1) * 8] = np.asarray(vec, np.float32).reshape(8, 128).T

    ng = inp["norm_g"]
    for l in range(2):
        for j in range(4):
            put(4 * l + j, ng[l, j])
    for i in range(6):
        put(8 + i, inp["mu"][0, i])
    put(14, inp["w_decay0"][0]); put(15, inp["a0"][0]); put(16, inp["k_k"][0]); put(17, inp["k_a"][0])
    put(18, inp["r_k"][0].reshape(-1)); put(19, inp["gn_g"][0]); put(20, inp["gn_b"][0]); put(21, inp["kv_g"])
    for l in range(2):
        for k in range(3):
            p[:, 176 + (l * 3 + k) * 44: 176 + (l * 3 + k + 1) * 44] = inp["conv_w"][l, k].reshape(44, 128).T
        p[:, 440 + l * 44: 440 + (l + 1) * 44] = inp["conv_b"][l].reshape(44, 128).T
    p[:, 528:544] = np.broadcast_to(inp["sinks"][0][None, :], (128, 16))
    return p


_NC = None


def kernel(**inputs):
    global _NC
    inp = {k: np.asarray(v) for k, v in inputs.items()}
    if _NC is None:
        _NC = build()
    cst = make_consts()
    par = make_par(inp)
    shared = {
        "cst": cst, "par": par,
        "w_rkv": np.ascontiguousarray(inp["w_rkv"][0]), "w_decay1": np.ascontiguousarray(inp["w_decay1"][0]),
        "w_decay2": np.ascontiguousarray(inp["w_decay2"][0]), "a1": np.ascontiguousarray(inp["a1"][0]),
        "a2": np.ascontiguousarray(inp["a2"][0]), "g1": np.ascontiguousarray(inp["g1"][0]),
        "g2": np.ascontiguousarray(inp["g2"][0]), "w_o_rwkv": np.ascontiguousarray(inp["w_o_rwkv"][0]),
        "w_kv": np.ascontiguousarray(inp["w_kv"]), "w_q": np.ascontiguousarray(inp["w_q"][0]),
        "w_o_attn": np.ascontiguousarray(inp["w_o_attn"][0]), "w_up": np.ascontiguousarray(inp["w_up"]),
        "w_down": np.ascontiguousarray(inp["w_down"]),
    }
    x = inp["x"].astype(np.float32)
    in_maps = [dict(shared, x=np.ascontiguousarray(x[i])) for i in range(8)]
    res = run_bass_kernel_spmd(_NC, in_maps, core_ids=list(range(8)))
    return np.stack([res.results[i]["y"] for i in range(8)], axis=0).astype(np.float32)
```
